# Optimizing a Trainium2 kernel written in Bass

```python
import math
import numpy as np
import jax
import jax.numpy as jnp
from jax import lax

D_MODEL = 1024
BATCH = 16
SEQ = 4096
DEPTH = 4

CHUNK = 64
N_BRANCH = 4
BRANCH_WIDTH = D_MODEL // 2
HEAD_DIM = 64
RMS_EPS = 1e-6

SSD_INNER = BRANCH_WIDTH
SSD_HEAD_DIM = HEAD_DIM
SSD_HEADS = SSD_INNER // SSD_HEAD_DIM
SSD_GROUPS = 2
SSD_STATE = 64
SSD_CONV = 4
SSD_XBC = SSD_INNER + 2 * SSD_GROUPS * SSD_STATE

DSA_WIDTH = BRANCH_WIDTH
DSA_HEAD_DIM = HEAD_DIM
DSA_HEADS = DSA_WIDTH // DSA_HEAD_DIM
IDX_HEADS = 4
IDX_DIM = 32
DSA_MAX_TOPK = 256
DSA_Q_BLOCK = 32

REL_BUCKETS = 32
REL_MAX_DIST = 256

RWKV_WIDTH = BRANCH_WIDTH
RWKV_HEAD_DIM = HEAD_DIM
RWKV_HEADS = RWKV_WIDTH // RWKV_HEAD_DIM
RWKV_DECAY_LORA = 64
RWKV_ICLR_LORA = 64
RWKV_GATE_LORA = 128
RWKV_GN_EPS = 64e-5
RWKV_COLS = 3 * RWKV_WIDTH + RWKV_DECAY_LORA + RWKV_ICLR_LORA + RWKV_GATE_LORA

S5_WIDTH = BRANCH_WIDTH
S5_GROUP = 16
S5_GROUPS = S5_WIDTH // S5_GROUP
S5_STATE = 64

FFN_HIDDEN = -(-8 * D_MODEL // (3 * 256)) * 256

IN_SPLITS = (
    SSD_INNER, SSD_XBC, SSD_HEADS,
    DSA_WIDTH, DSA_WIDTH, DSA_WIDTH, IDX_HEADS * IDX_DIM, IDX_DIM, IDX_HEADS,
    RWKV_COLS,
    S5_WIDTH,
    N_BRANCH * D_MODEL,
)
IN_COLS = sum(IN_SPLITS)

kernel_name = "chunk_causal_hybrid_ssd_dsa_rwkv7_s5"


def _split_last(a, sizes):
    idx = np.cumsum(sizes)[:-1].tolist()
    return jnp.split(a, idx, axis=-1)


def _rmsnorm(x, g):
    xf = x.astype(jnp.float32)
    y = xf * lax.rsqrt(jnp.mean(xf * xf, axis=-1, keepdims=True) + RMS_EPS)
    return (y * g.astype(jnp.float32)).astype(x.dtype)


def _causal_depthwise_conv(x, w, bias):
    width, ch = w.shape
    y = lax.conv_general_dilated(x, w[:, None, :].astype(x.dtype), window_strides=(1,),
                                 padding=((width - 1, 0),), dimension_numbers=("NWC", "WIO", "NWC"),
                                 feature_group_count=ch)
    return y + bias.astype(y.dtype)


def _segsum(a):
    n = a.shape[-1]
    cs = jnp.cumsum(a, axis=-1)
    diff = cs[..., :, None] - cs[..., None, :]
    return jnp.where(jnp.tril(jnp.ones((n, n), dtype=bool)), diff, -jnp.inf)


def _ssd_scan(xs, a, bm, cm):
    b, t, h, p = xs.shape
    n = bm.shape[-1]
    c = t // CHUNK
    xs = xs.reshape(b, c, CHUNK, h, p)
    bm = bm.reshape(b, c, CHUNK, h, n)
    cm = cm.reshape(b, c, CHUNK, h, n)
    a = a.reshape(b, c, CHUNK, h).transpose(0, 3, 1, 2)
    a_cs = jnp.cumsum(a, axis=-1)
    decay_in = jnp.exp(_segsum(a))
    y_diag = jnp.einsum("bclhn,bcshn,bhcls,bcshp->bclhp", cm, bm, decay_in, xs)
    decay_to_end = jnp.exp(a_cs[..., -1:] - a_cs)
    chunk_states = jnp.einsum("bclhn,bhcl,bclhp->bchpn", bm, decay_to_end, xs)
    chunk_decay = jnp.exp(a_cs[..., -1])

    def step(state, inp):
        st, dec = inp
        return state * dec[..., None, None] + st, state

    _, prev = lax.scan(step, jnp.zeros((b, h, p, n), xs.dtype),
                       (jnp.moveaxis(chunk_states, 1, 0), jnp.moveaxis(chunk_decay, 2, 0)))
    prev = jnp.moveaxis(prev, 0, 1)
    y_off = jnp.einsum("bclhn,bchpn,bhcl->bclhp", cm, prev, jnp.exp(a_cs))
    return (y_diag + y_off).reshape(b, t, h, p)


def _ssd_mixer(z, xbc, dt_raw, conv_w, conv_b, dt_bias, a_log, d_skip, norm_g):
    b, t, _ = z.shape
    f32 = jnp.float32
    xbc = jax.nn.silu(_causal_depthwise_conv(xbc, conv_w, conv_b)).astype(f32)
    xs, bm, cm = _split_last(xbc, (SSD_INNER, SSD_GROUPS * SSD_STATE, SSD_GROUPS * SSD_STATE))
    xs = xs.reshape(b, t, SSD_HEADS, SSD_HEAD_DIM)
    rep = SSD_HEADS // SSD_GROUPS
    bm = jnp.repeat(bm.reshape(b, t, SSD_GROUPS, SSD_STATE), rep, axis=2)
    cm = jnp.repeat(cm.reshape(b, t, SSD_GROUPS, SSD_STATE), rep, axis=2)
    dt = jax.nn.softplus(dt_raw.astype(f32) + dt_bias.astype(f32))
    a = -jnp.exp(a_log.astype(f32))
    y = _ssd_scan(xs * dt[..., None], dt * a, bm, cm)
    y = y + d_skip.astype(f32)[:, None] * xs
    y = y.reshape(b, t, SSD_INNER) * jax.nn.silu(z.astype(f32))
    yg = y.reshape(b, t, SSD_GROUPS, SSD_INNER // SSD_GROUPS)
    yg = yg * lax.rsqrt(jnp.mean(yg * yg, axis=-1, keepdims=True) + RMS_EPS)
    return (yg.reshape(b, t, SSD_INNER) * norm_g.astype(f32)).astype(z.dtype)


def _t5_bucket(rel):
    half = REL_BUCKETS // 2
    exact = half // 2
    ret = jnp.where(rel > 0, half, 0)
    n = jnp.abs(rel)
    n_f = jnp.maximum(n, 1).astype(jnp.float32)
    large = exact + (jnp.log(n_f / exact) / math.log(REL_MAX_DIST / exact) * (half - exact)).astype(jnp.int32)
    large = jnp.minimum(large, half - 1)
    return ret + jnp.where(n < exact, n, large)


def _dsa_mixer(q, k, v, qi, ki, wi, rel_bias):
    b, t, h, dh = q.shape
    f32 = jnp.float32
    top_k = min(DSA_MAX_TOPK, t // 4)
    n_blk = t // DSA_Q_BLOCK
    key_chunk = jnp.arange(t) // CHUNK
    ki_f = ki.astype(f32)

    def one_block(start):
        qb = lax.dynamic_slice_in_dim(q, start, DSA_Q_BLOCK, axis=1)
        qib = lax.dynamic_slice_in_dim(qi, start, DSA_Q_BLOCK, axis=1).astype(f32)
        wib = lax.dynamic_slice_in_dim(wi, start, DSA_Q_BLOCK, axis=1).astype(f32)
        q_pos = start + jnp.arange(DSA_Q_BLOCK)
        s_h = jax.nn.relu(jnp.einsum("bqhe,bse->bqhs", qib, ki_f) * IDX_DIM ** -0.5)
        score = jnp.einsum("bqhs,bqh->bqs", s_h, wib) * IDX_HEADS ** -0.5
        admissible = key_chunk[None, :] <= (q_pos // CHUNK)[:, None]
        score = jnp.where(admissible[None], score, -jnp.inf)
        sel_score, sel_idx = lax.top_k(score, top_k)
        valid = jnp.isfinite(sel_score)
        k_sel = jax.vmap(lambda kb, ib: kb[ib])(k, sel_idx)
        v_sel = jax.vmap(lambda vb, ib: vb[ib])(v, sel_idx)
        logits = jnp.einsum("bqhd,bqkhd->bqhk", qb, k_sel).astype(f32) * dh ** -0.5
        bucket = _t5_bucket(sel_idx - q_pos[None, :, None])
        logits = logits + jnp.transpose(rel_bias[bucket], (0, 1, 3, 2)).astype(f32)
        logits = jnp.where(valid[:, :, None, :], logits, -jnp.inf)
        probs = jax.nn.softmax(logits, axis=-1)
        return jnp.einsum("bqhk,bqkhd->bqhd", probs.astype(v.dtype), v_sel)

    out = lax.map(one_block, jnp.arange(n_blk, dtype=jnp.int32) * DSA_Q_BLOCK)
    return out.transpose(1, 0, 2, 3, 4).reshape(b, t, h * dh)


def _wkv7_scan(r, decay, k, v, a, bb):
    bsz, t, h, n = r.shape

    def step(s, inp):
        r_t, w_t, k_t, v_t, a_t, b_t = inp
        sa = jnp.einsum("bhvk,bhk->bhv", s, a_t)
        s = s * w_t[:, :, None, :] + sa[..., None] * b_t[:, :, None, :] + v_t[..., None] * k_t[:, :, None, :]
        return s, jnp.einsum("bhvk,bhk->bhv", s, r_t)

    seq = tuple(jnp.moveaxis(z, 1, 0) for z in (r, decay, k, v, a, bb))
    _, y = lax.scan(step, jnp.zeros((bsz, h, n, n), jnp.float32), seq)
    return jnp.moveaxis(y, 0, 1)


def _rwkv7_mixer(cols, mu, w0, w2, a0, a2, g2, k_k, k_a, r_k, ln_g, ln_b):
    b, t, _ = cols.shape
    f32 = jnp.float32
    hd = (b, t, RWKV_HEADS, RWKV_HEAD_DIM)
    p = cols.astype(f32)
    p_prev = jnp.pad(p, ((0, 0), (1, 0), (0, 0)))[:, :-1]
    p = p + (p_prev - p) * mu.astype(f32)
    r, k, v, dw, da, dg = _split_last(p, (RWKV_WIDTH, RWKV_WIDTH, RWKV_WIDTH,
                                          RWKV_DECAY_LORA, RWKV_ICLR_LORA, RWKV_GATE_LORA))
    w = -jax.nn.softplus(-(w0.astype(f32) + jnp.tanh(dw) @ w2.astype(f32))) - 0.5
    decay = jnp.exp(-jnp.exp(w))
    a = jax.nn.sigmoid(a0.astype(f32) + da @ a2.astype(f32))
    g = jax.nn.sigmoid(dg) @ g2.astype(f32)
    kk = (k * k_k.astype(f32)).reshape(hd)
    kk = kk / jnp.maximum(jnp.linalg.norm(kk, axis=-1, keepdims=True), 1e-12)
    k = k * (1.0 + (a - 1.0) * k_a.astype(f32))
    rh, kh, vh, ah = r.reshape(hd), k.reshape(hd), v.reshape(hd), a.reshape(hd)
    y = _wkv7_scan(rh, decay.reshape(hd), kh, vh, -kk, kk * ah)
    mean = jnp.mean(y, axis=-1, keepdims=True)
    var = jnp.mean(jnp.square(y - mean), axis=-1, keepdims=True)
    y = ((y - mean) * lax.rsqrt(var + RWKV_GN_EPS)).reshape(b, t, RWKV_WIDTH)
    y = y * ln_g.astype(f32) + ln_b.astype(f32)
    bonus = jnp.sum(rh * kh * r_k.astype(f32), axis=-1, keepdims=True) * vh
    y = y + bonus.reshape(b, t, RWKV_WIDTH)
    return (y * g).astype(cols.dtype)


def _s5_mixer(u, a_re, a_im, b_re, b_im, c_re, c_im, d_skip, log_dt, glu_w, glu_b):
    bsz, t, _ = u.shape
    f32 = jnp.float32
    uf = u.astype(f32)
    ug = uf.reshape(bsz, t, S5_GROUPS, S5_GROUP)
    a = lax.complex(a_re.astype(f32), a_im.astype(f32))
    dt = jnp.exp(log_dt.astype(f32))[:, None]
    a_bar = jnp.exp(a * dt)
    b_bar = ((a_bar - 1.0) / a)[..., None] * lax.complex(b_re.astype(f32), b_im.astype(f32))
    bu = lax.complex(jnp.einsum("btgj,gpj->btgp", ug, jnp.real(b_bar)),
                     jnp.einsum("btgj,gpj->btgp", ug, jnp.imag(b_bar)))

    def combine(e1, e2):
        a1, b1 = e1
        a2, b2 = e2
        return a1 * a2, a2 * b1 + b2

    a_seq = jnp.broadcast_to(a_bar, (1, t, S5_GROUPS, S5_STATE))
    _, states = lax.associative_scan(combine, (a_seq, bu), axis=1)
    y = (jnp.einsum("btgp,gjp->btgj", jnp.real(states), c_re.astype(f32))
         - jnp.einsum("btgp,gjp->btgj", jnp.imag(states), c_im.astype(f32)))
    y = y.reshape(bsz, t, S5_WIDTH) + d_skip.astype(f32) * uf
    y = jax.nn.gelu(y)
    y = y * jax.nn.sigmoid(y @ glu_w.astype(f32) + glu_b.astype(f32))
    return y.astype(u.dtype)


def _swiglu(h, w1, w3, w2):
    return (jax.nn.silu(h @ w1) * (h @ w3)) @ w2


def setup_inputs(seed: int = 0) -> dict:
    key = jax.random.key(seed)
    ks = iter(jax.random.split(key, 40))
    f32 = jnp.float32
    L = DEPTH

    def nrm(shape, scale):
        return jax.random.normal(next(ks), shape, f32) * scale

    def uni(shape, lo, hi):
        return jax.random.uniform(next(ks), shape, f32, lo, hi)

    x = nrm((BATCH, SEQ, D_MODEL), 1.0)
    rel_bias = nrm((REL_BUCKETS, DSA_HEADS), 0.5)
    norm_mix_g = 1.0 + nrm((L, D_MODEL), 0.02)
    w_in = nrm((L, D_MODEL, IN_COLS), D_MODEL ** -0.5)
    ssd_conv_w = nrm((L, SSD_CONV, SSD_XBC), SSD_CONV ** -0.5)
    ssd_conv_b = nrm((L, SSD_XBC), 0.02)
    dt0 = jnp.exp(uni((L, SSD_HEADS), math.log(1e-3), math.log(1e-1)))
    ssd_dt_bias = dt0 + jnp.log(-jnp.expm1(-dt0))
    ssd_a_log = jnp.log(uni((L, SSD_HEADS), 1.0, 16.0))
    ssd_d = 1.0 + nrm((L, SSD_HEADS), 0.1)
    ssd_norm_g = 1.0 + nrm((L, SSD_INNER), 0.02)
    rwkv_mu = uni((L, RWKV_COLS), 0.0, 1.0)
    rwkv_w0 = uni((L, RWKV_WIDTH), -6.0, -1.0)
    rwkv_w2 = nrm((L, RWKV_DECAY_LORA, RWKV_WIDTH), 0.1)
    rwkv_a0 = nrm((L, RWKV_WIDTH), 0.1)
    rwkv_a2 = nrm((L, RWKV_ICLR_LORA, RWKV_WIDTH), 0.1)
    rwkv_g2 = nrm((L, RWKV_GATE_LORA, RWKV_WIDTH), RWKV_GATE_LORA ** -0.5)
    rwkv_k_k = 0.85 + nrm((L, RWKV_WIDTH), 0.05)
    rwkv_k_a = 1.0 + nrm((L, RWKV_WIDTH), 0.05)
    rwkv_r_k = nrm((L, RWKV_HEADS, RWKV_HEAD_DIM), 0.1)
    rwkv_ln_g = 1.0 + nrm((L, RWKV_WIDTH), 0.02)
    rwkv_ln_b = nrm((L, RWKV_WIDTH), 0.02)
    s5_a_re = -0.5 + nrm((L, S5_GROUPS, S5_STATE), 0.01)
    s5_a_im = jnp.pi * jnp.arange(S5_STATE, dtype=f32) + nrm((L, S5_GROUPS, S5_STATE), 0.01)
    s5_b_re = nrm((L, S5_GROUPS, S5_STATE, S5_GROUP), (2 * S5_GROUP) ** -0.5)
    s5_b_im = nrm((L, S5_GROUPS, S5_STATE, S5_GROUP), (2 * S5_GROUP) ** -0.5)
    s5_c_re = nrm((L, S5_GROUPS, S5_GROUP, S5_STATE), 0.25)
    s5_c_im = nrm((L, S5_GROUPS, S5_GROUP, S5_STATE), 0.25)
    s5_d = nrm((L, S5_WIDTH), 1.0)
    s5_log_dt = uni((L, S5_GROUPS), math.log(1e-3), math.log(1e-1))
    s5_glu_w = nrm((L, S5_WIDTH, S5_WIDTH), S5_WIDTH ** -0.5)
    s5_glu_b = nrm((L, S5_WIDTH), 0.02)
    w_branch = nrm((L, N_BRANCH, BRANCH_WIDTH, D_MODEL), BRANCH_WIDTH ** -0.5)
    w_out = nrm((L, D_MODEL, D_MODEL), D_MODEL ** -0.5)
    norm_ffn_g = 1.0 + nrm((L, D_MODEL), 0.02)
    ffn_w1 = nrm((L, D_MODEL, FFN_HIDDEN), D_MODEL ** -0.5)
    ffn_w3 = nrm((L, D_MODEL, FFN_HIDDEN), D_MODEL ** -0.5)
    ffn_w2 = nrm((L, FFN_HIDDEN, D_MODEL), FFN_HIDDEN ** -0.5)
    norm_final_g = 1.0 + nrm((D_MODEL,), 0.02)
    return {
        "x": x, "rel_bias": rel_bias, "norm_mix_g": norm_mix_g, "w_in": w_in,
        "ssd_conv_w": ssd_conv_w, "ssd_conv_b": ssd_conv_b, "ssd_dt_bias": ssd_dt_bias,
        "ssd_a_log": ssd_a_log, "ssd_d": ssd_d, "ssd_norm_g": ssd_norm_g,
        "rwkv_mu": rwkv_mu, "rwkv_w0": rwkv_w0, "rwkv_w2": rwkv_w2, "rwkv_a0": rwkv_a0,
        "rwkv_a2": rwkv_a2, "rwkv_g2": rwkv_g2, "rwkv_k_k": rwkv_k_k, "rwkv_k_a": rwkv_k_a,
        "rwkv_r_k": rwkv_r_k, "rwkv_ln_g": rwkv_ln_g, "rwkv_ln_b": rwkv_ln_b,
        "s5_a_re": s5_a_re, "s5_a_im": s5_a_im, "s5_b_re": s5_b_re, "s5_b_im": s5_b_im,
        "s5_c_re": s5_c_re, "s5_c_im": s5_c_im, "s5_d": s5_d, "s5_log_dt": s5_log_dt,
        "s5_glu_w": s5_glu_w, "s5_glu_b": s5_glu_b, "w_branch": w_branch, "w_out": w_out,
        "norm_ffn_g": norm_ffn_g, "ffn_w1": ffn_w1, "ffn_w3": ffn_w3, "ffn_w2": ffn_w2,
        "norm_final_g": norm_final_g,
    }


def reference(x, rel_bias, norm_mix_g, w_in, ssd_conv_w, ssd_conv_b, ssd_dt_bias, ssd_a_log, ssd_d,
              ssd_norm_g, rwkv_mu, rwkv_w0, rwkv_w2, rwkv_a0, rwkv_a2, rwkv_g2, rwkv_k_k, rwkv_k_a,
              rwkv_r_k, rwkv_ln_g, rwkv_ln_b, s5_a_re, s5_a_im, s5_b_re, s5_b_im, s5_c_re, s5_c_im,
              s5_d, s5_log_dt, s5_glu_w, s5_glu_b, w_branch, w_out, norm_ffn_g, ffn_w1, ffn_w3,
              ffn_w2, norm_final_g):
    b, t, _ = x.shape
    f32 = jnp.float32
    for i in range(DEPTH):
        h = _rmsnorm(x, norm_mix_g[i])
        proj = h @ w_in[i]
        (z, xbc, dt_raw, q, k, v, qi, ki, wi, rwkv_cols, s5_u, gate_cols) = _split_last(proj, IN_SPLITS)
        y_a = _ssd_mixer(z, xbc, dt_raw, ssd_conv_w[i], ssd_conv_b[i], ssd_dt_bias[i],
                         ssd_a_log[i], ssd_d[i], ssd_norm_g[i])
        y_b = _dsa_mixer(q.reshape(b, t, DSA_HEADS, DSA_HEAD_DIM), k.reshape(b, t, DSA_HEADS, DSA_HEAD_DIM),
                         v.reshape(b, t, DSA_HEADS, DSA_HEAD_DIM), qi.reshape(b, t, IDX_HEADS, IDX_DIM),
                         ki, wi, rel_bias).astype(x.dtype)
        y_c = _rwkv7_mixer(rwkv_cols, rwkv_mu[i], rwkv_w0[i], rwkv_w2[i], rwkv_a0[i], rwkv_a2[i],
                           rwkv_g2[i], rwkv_k_k[i], rwkv_k_a[i], rwkv_r_k[i], rwkv_ln_g[i], rwkv_ln_b[i])
        y_d = _s5_mixer(s5_u, s5_a_re[i], s5_a_im[i], s5_b_re[i], s5_b_im[i], s5_c_re[i], s5_c_im[i],
                        s5_d[i], s5_log_dt[i], s5_glu_w[i], s5_glu_b[i])
        merged = jnp.zeros((b, t, D_MODEL), f32)
        for n, y_n in enumerate((y_a, y_b, y_c, y_d)):
            gate = jax.nn.sigmoid(gate_cols[..., n * D_MODEL:(n + 1) * D_MODEL].astype(f32))
            merged = merged + gate * (y_n @ w_branch[i, n]).astype(f32)
        x = x + (merged.astype(x.dtype) @ w_out[i]).astype(x.dtype)
        h = _rmsnorm(x, norm_ffn_g[i])
        x = x + _swiglu(h, ffn_w1[i], ffn_w3[i], ffn_w2[i]).astype(x.dtype)
    return _rmsnorm(x, norm_final_g)
```

```python
import numpy as np
import ml_dtypes
import concourse.bass as bass
import concourse.mybir as mybir
from concourse.alu_op_type import AluOpType as ALU
from concourse.bass_utils import run_bass_kernel_spmd
from contextlib import ExitStack

F32 = mybir.dt.float32
BF16 = mybir.dt.bfloat16
AF = mybir.ActivationFunctionType
AX = mybir.AxisListType

D = 1024
NCOL = 9388
FH = 2816
EPS = 1e-6
PV_A, PV_D, PV_C, NPV = 0, 80, 136, 192
C_Z, C_XBC, C_DT, C_Q, C_K, C_V, C_QI, C_KI, C_WI, C_RW, C_S5, C_G = 0, 512, 1280, 1288, 1800, 2312, 2824, 2952, 2984, 2988, 4780, 5292


def t5_bucket_np(rel):
    rel = np.asarray(rel, np.int32)
    half, exact = 16, 8
    ret = np.where(rel > 0, half, 0)
    n = np.abs(rel)
    n_f = np.maximum(n, 1).astype(np.float32)
    large = exact + (np.log(n_f / np.float32(exact)) / np.float32(np.log(256 / exact)) * np.float32(half - exact)).astype(np.int32)
    large = np.minimum(large, half - 1)
    return ret + np.where(n < exact, n, large)


def make_oh():
    lst, tiles = [], []
    s_ = np.arange(128)[:, None]
    t_ = np.arange(128)[None, :]
    for c in range(3):
        bk = t5_bucket_np(s_ - t_ - 128 * c)
        for v in np.unique(bk):
            lst.append((c, int(v)))
            tiles.append((bk == v).astype(np.float32))
    return lst, np.ascontiguousarray(np.stack(tiles, 1))


OH_LIST, OH_TILES = make_oh()


class KB:
    def __init__(self, nc, es, ndma=90):
        self.nc = nc
        self.E = {'pe': nc.tensor, 'dve': nc.vector, 'act': nc.scalar, 'pool': nc.gpsimd, 'sp': nc.sync}
        self.sems, self.cnt = {}, {}
        for e in self.E:
            self.sems[e] = es.enter_context(nc.semaphore('S_' + e))
            self.cnt[e] = 0
        self.pool_sems = []
        for i in range(ndma):
            k = 'D%d' % i
            self.sems[k] = es.enter_context(nc.semaphore('S_' + k))
            self.cnt[k] = 0
            self.pool_sems.append(k)
        self.dmap = {}
        self.seen = {e: {} for e in self.E}
        self.last_w, self.readers = {}, {}
        self.uid = 0
        self.ninstr = 0

    def key(self, b):
        return b if isinstance(b, (str, tuple)) else b.name

    def deps(self, r, w):
        d = []
        for b in r:
            k = self.key(b)
            if k in self.last_w:
                d.append(self.last_w[k])
        for b in w:
            k = self.key(b)
            if k in self.last_w:
                d.append(self.last_w[k])
            d.extend(self.readers.get(k, {}).items())
        return d

    def wait(self, eng, deps):
        best = {}
        for sk, v in deps:
            if v > best.get(sk, 0):
                best[sk] = v
        for sk, v in best.items():
            if eng == 'pe' and sk == 'pe':
                continue
            if self.seen[eng].get(sk, 0) < v:
                self.E[eng].wait_ge(self.sems[sk], v)
                self.seen[eng][sk] = v
                self.ninstr += 1

    def record(self, tok, r, w):
        for b in r:
            rd = self.readers.setdefault(self.key(b), {})
            if rd.get(tok[0], 0) < tok[1]:
                rd[tok[0]] = tok[1]
        for b in w:
            k = self.key(b)
            self.last_w[k] = tok
            self.readers[k] = {}

    def op(self, eng, fn, r=(), w=()):
        w = tuple(w) + tuple(b for b in r if self.key(b).startswith('ps'))
        self.wait(eng, self.deps(r, w))
        ins = fn(self.E[eng])
        self.cnt[eng] += 1
        ins.then_inc(self.sems[eng], 1)
        self.record((eng, self.cnt[eng]), r, w)
        self.ninstr += 1

    def dma(self, q, out, in_, sb, r=(), w=()):
        k0 = self.key(sb)
        if k0 not in self.dmap:
            assert len(self.dmap) < len(self.pool_sems), "out of dma sems"
            self.dmap[k0] = self.pool_sems[len(self.dmap)]
        k = self.dmap[k0]
        self.wait(q, self.deps(r, w))
        ins = self.E[q].dma_start(out=out, in_=in_)
        self.cnt[k] += 16
        ins.then_inc(self.sems[k], 16)
        self.record((k, self.cnt[k]), r, w)
        self.ninstr += 1

    def load(self, out, in_, sb, q='sp'):
        self.dma(q, out, in_, sb, r=(), w=(sb,))

    def store(self, out, in_, sb, q='pool'):
        self.dma(q, out, in_, sb, r=(sb,), w=())

    def barrier(self):
        for e in self.E:
            for sk, c in self.cnt.items():
                if e == sk:
                    continue
                if self.seen[e].get(sk, 0) < c:
                    self.E[e].wait_ge(self.sems[sk], c)
                    self.seen[e][sk] = c
                    self.ninstr += 1
        self.dmap = {}
        self.last_w, self.readers = {}, {}

    def name(self, p):
        self.uid += 1
        return '%s_%d' % (p, self.uid)


class Cfg:
    def __init__(self, T=4096, NL=4, feed=(), dump=(), phases=None, stop=0):
        self.stop = stop
        self.T, self.NL, self.feed, self.dump = T, NL, set(feed), set(dump)
        self.phases = phases
        self.NBLK = T // 512


def build(cfg):
    nc = bass.Bass("TRN2", target_bir_lowering=False)
    T, NL = cfg.T, cfg.NL
    with ExitStack() as es:
        kb = KB(nc, es)
        B = Builder(nc, kb, cfg, es)
        B.run()
    return nc, B


class Builder:
    def __init__(self, nc, kb, cfg, es):
        self.nc, self.kb, self.cfg, self.es = nc, kb, cfg, es
        self.ext = {}
        self.outs = []

    def din(self, name, shape, dt=F32):
        t = self.nc.dram_tensor(name, list(shape), dt, kind="ExternalInput")
        self.ext[name] = (tuple(shape), dt)
        return t.ap()

    def dscr(self, name, shape, dt):
        if name in self.cfg.feed:
            return self.din(name, shape, dt)
        if name in self.cfg.dump:
            self.outs.append(name)
            return self.nc.dram_tensor(name, list(shape), dt, kind="ExternalOutput").ap()
        return self.nc.dram_tensor(name, list(shape), dt, kind="Internal").ap()

    def sb(self, st, p, shape, dt):
        return st.enter_context(self.nc.sbuf_tensor(self.kb.name(p), list(shape), dt))

    def run(self):
        nc, kb, cfg = self.nc, self.kb, self.cfg
        T, NL = cfg.T, cfg.NL
        dense = cfg.phases is None or any(p in cfg.phases for p in ('setup', 'P', 'M'))
        self.x_in = self.din("xT", [2, D, T])
        if dense:
            self.w_in = self.din("w_in", [NL, D, NCOL])
            self.w_br = self.din("w_branch", [NL, 4, 512, D])
            self.w_out = self.din("w_out", [NL, D, D])
            self.w1 = self.din("ffn_w1", [NL, D, FH])
            self.w3 = self.din("ffn_w3", [NL, D, FH])
            self.w2 = self.din("ffn_w2", [NL, FH, D])
        self.gvec = self.din("gvec", [128, NL * 16 + 8])
        self.cst_f = self.din("cst_f32", [128, 512])
        self.pvec = self.din("pvec", [NL, 128, NPV])
        self.cst_l = self.din("cst_l", [128, 512])
        self.cst_oh = self.din("cst_oh", [128, len(OH_LIST), 128])
        self.relb = self.din("relb", [128, 256])
        self.cst_bd = self.din("cst_bd", [128, 128])
        self.cst_sel = self.din("cst_sel", [128, 64, 128])
        self.rw_w2 = self.din("rw_w2", [NL, 64, 512])
        self.rw_a2 = self.din("rw_a2", [NL, 64, 512])
        self.rw_g2 = self.din("rw_g2", [NL, 128, 512])
        self.s_rv = self.dscr("s_rv", [2, 5, 512, T], F32)
        self.s_rx = self.dscr("s_rx", [2, 3, 512, T], F32)
        self.s_ry = self.dscr("s_ry", [2, 512, T], F32)
        self.s5wb = self.din("s5wb", [NL, 2, 16, 128, 128])
        self.s5wc = self.din("s5wc", [NL, 2, 16, 128, 128])
        self.s5glu = self.din("s5glu", [NL, 512, 512])
        self.out = self.nc.dram_tensor("outT", [2, D, T], F32, kind="ExternalOutput").ap()
        self.xr = self.dscr("xr", [2, D, T], F32)
        self.wb_in = self.dscr("wb_in", [NL, D, NCOL], BF16)
        self.wb_br = self.dscr("wb_br", [NL, 4, 512, D], BF16)
        self.wb_out = self.dscr("wb_out", [NL, D, D], BF16)
        self.wb_1 = self.dscr("wb_1", [NL, D, FH], BF16)
        self.wb_3 = self.dscr("wb_3", [NL, D, FH], BF16)
        self.wb_2 = self.dscr("wb_2", [NL, FH, D], BF16)
        self.s_z = self.dscr("s_z", [2, 64, 8, T], F32)
        self.s_xbc = self.dscr("s_xbc", [2, 768, T], F32)
        self.s_dt = self.dscr("s_dt", [2, T, 8], F32)
        self.s_q = self.dscr("s_q", [2, 64, 8, T], BF16)
        self.s_k = self.dscr("s_k", [2, 64, 8, T], BF16)
        self.s_v = self.dscr("s_v", [2, T, 512], BF16)
        self.s_qi = self.dscr("s_qi", [2, 32, 4, T], BF16)
        self.s_ki = self.dscr("s_ki", [2, 32, T], BF16)
        self.s_wi = self.dscr("s_wi", [2, T, 4], F32)
        self.s_rw = self.dscr("s_rw", [2, 1792, T], F32)
        self.s_u5 = self.dscr("s_u5", [2, 512, T], F32)
        self.s_gs = self.dscr("s_gs", [2, 4096, T], BF16)
        self.s_y4 = self.dscr("s_y4", [2, 4, 512, T], BF16)

        with ExitStack() as st:
            self.cf = self.sb(st, "cf", [128, 512], F32)
            self.gv = self.sb(st, "gv", [128, NL * 16 + 8], F32)
            kb.load(self.cf[:], self.cst_f[:, :], self.cf)
            kb.load(self.gv[:], self.gvec[:, :], self.gv)
            self.ps = [st.enter_context(nc.psum_tensor(kb.name("ps"), [128, 512], F32)) for _ in range(6)]
            self.psT = [st.enter_context(nc.psum_tensor(kb.name("psT"), [128, 1024], BF16)) for _ in range(2)]
            self.psi = 0
            self.onesf = self.cf[:, 0:128]
            self.epsb = self.cf[:, 384:385]
            self.negpi = self.cf[:, 385:386]
            ph = cfg.phases
            if ph is None or 'setup' in ph:
                self.setup()
                kb.barrier()
            for l in range(NL):
                for b in range(2):
                    if ph is None or 'P' in ph:
                        self.phase_P(l, b)
                        kb.barrier()
                if ph is None or 'C' in ph:
                    for b in range(2):
                        self.phase_C1(l, b)
                        kb.barrier()
                    self.phase_C2(l)
                    kb.barrier()
                    for b in range(2):
                        self.phase_C3(l, b)
                        kb.barrier()
                for b in range(2):
                    for nm, fn in (('A', self.phase_A), ('D', self.phase_D), ('B', self.phase_B)):
                        if ph is None or nm in ph:
                            fn(l, b)
                            kb.barrier()
                for b in range(2):
                    if ph is None or 'M' in ph:
                        self.phase_M(l, b)
                        kb.barrier()
            kb.barrier()

    def nps(self):
        p = self.ps[self.psi % 6]
        self.psi += 1
        return p

    def setup(self):
        kb = self.kb
        NL = self.cfg.NL
        with ExitStack() as st:
            fb = [self.sb(st, "cfb", [128, 4096], F32) for _ in range(2)]
            bb = [self.sb(st, "cbb", [128, 4096], BF16) for _ in range(2)]
            i = 0
            engs = ['pool', 'act', 'dve']

            def cast2d(src, dst, R, C):
                nonlocal i
                rc = R // 128
                s3 = src.rearrange("(c p) n -> p c n", p=128)
                d3 = dst.rearrange("(c p) n -> p c n", p=128)
                cw = min(C, 4096)
                for c in range(rc):
                    for c0 in range(0, C, cw):
                        w = min(cw, C - c0)
                        f, bt = fb[i % 2], bb[i % 2]
                        kb.load(f[:, 0:w], s3[:, c, c0:c0 + w], f)
                        e = engs[i % 3]
                        if e == 'act':
                            kb.op('act', lambda E: E.activation(out=bt[:, 0:w], in_=f[:, 0:w], func=AF.Copy), r=(f,), w=(bt,))
                        else:
                            kb.op(e, lambda E: E.tensor_copy(out=bt[:, 0:w], in_=f[:, 0:w]), r=(f,), w=(bt,))
                        kb.store(d3[:, c, c0:c0 + w], bt[:, 0:w], bt, q='act' if i % 2 else 'pool')
                        i += 1
            for l in range(NL):
                cast2d(self.w_in[l], self.wb_in[l], D, NCOL)
                for n in range(4):
                    cast2d(self.w_br[l, n], self.wb_br[l, n], 512, D)
                cast2d(self.w_out[l], self.wb_out[l], D, D)
                cast2d(self.w1[l], self.wb_1[l], D, FH)
                cast2d(self.w3[l], self.wb_3[l], D, FH)
                cast2d(self.w2[l], self.wb_2[l], FH, D)

    def rmsnorm_fm(self, st, xT, gcol0, hT, out_f32=None):
        kb = self.kb
        sq = self.sb(st, "sq", [128, 8, 512], F32)
        rstd = self.sb(st, "rstd", [128, 512], F32)
        kb.op('act', lambda E: E.activation(out=sq[:], in_=xT[:], func=AF.Square), r=(xT,), w=(sq,))
        ps = self.nps()
        for kc in range(8):
            kb.op('pe', lambda E: E.matmul(ps[:], lhsT=self.onesf, rhs=sq[:, kc, :], start=(kc == 0), stop=(kc == 7)), r=(sq, self.cf), w=(ps,))
        kb.op('act', lambda E: E.activation(out=rstd[:], in_=ps[:], func=AF.Sqrt, bias=self.epsb, scale=1.0 / D), r=(ps, self.cf), w=(rstd,))
        kb.op('dve', lambda E: E.reciprocal(out=rstd[:], in_=rstd[:]), r=(rstd,), w=(rstd,))
        for kc in range(8):
            o = hT if out_f32 is None else out_f32
            kb.op('dve', lambda E: E.scalar_tensor_tensor(out=o[:, kc, :], in0=xT[:, kc, :], scalar=self.gv[:, gcol0 + kc:gcol0 + kc + 1],
                                                          in1=rstd[:], op0=ALU.mult, op1=ALU.mult), r=(xT, rstd, self.gv), w=(o,))

    def phase_P(self, l, b):
        kb, cfg = self.kb, self.cfg
        T = cfg.T
        xsrc = self.x_in if l == 0 else self.xr
        wsrc = self.wb_in[l].rearrange("(c p) n -> p c n", p=128)
        ents = []
        for h in range(8):
            ents.append((C_Z + 64 * h, 64, (lambda t0, h=h: self.s_z[b, :, h, t0:t0 + 512]), F32, AF.Copy))
        for i in range(6):
            ents.append((C_XBC + 128 * i, 128, (lambda t0, i=i: self.s_xbc[b, 128 * i:128 * i + 128, t0:t0 + 512]), F32, AF.Copy))
        for h in range(8):
            ents.append((C_Q + 64 * h, 64, (lambda t0, h=h: self.s_q[b, :, h, t0:t0 + 512]), BF16, AF.Copy))
        for h in range(8):
            ents.append((C_K + 64 * h, 64, (lambda t0, h=h: self.s_k[b, :, h, t0:t0 + 512]), BF16, AF.Copy))
        for h in range(4):
            ents.append((C_QI + 32 * h, 32, (lambda t0, h=h: self.s_qi[b, :, h, t0:t0 + 512]), BF16, AF.Copy))
        ents.append((C_KI, 32, (lambda t0: self.s_ki[b, :, t0:t0 + 512]), BF16, AF.Copy))
        for i in range(14):
            ents.append((C_RW + 128 * i, 128, (lambda t0, i=i: self.s_rw[b, 128 * i:128 * i + 128, t0:t0 + 512]), F32, AF.Copy))
        for i in range(4):
            ents.append((C_S5 + 128 * i, 128, (lambda t0, i=i: self.s_u5[b, 128 * i:128 * i + 128, t0:t0 + 512]), F32, AF.Copy))
        for i in range(32):
            ents.append((C_G + 128 * i, 128, (lambda t0, i=i: self.s_gs[b, 128 * i:128 * i + 128, t0:t0 + 512]), BF16, AF.Sigmoid))
        tms = [(C_V, 512, (lambda t0: self.s_v[b, t0:t0 + 128, :]), BF16),
               (C_DT, 8, (lambda t0: self.s_dt[b, t0:t0 + 128, :]), F32),
               (C_WI, 4, (lambda t0: self.s_wi[b, t0:t0 + 128, :]), F32)]
        with ExitStack() as st:
            wt = [self.sb(st, "wt", [128, 8, 512], BF16) for _ in range(2)]
            xT = self.sb(st, "xT", [128, 8, 512], F32)
            hT = [self.sb(st, "hT", [128, 8, 512], BF16) for _ in range(cfg.NBLK)]
            of = [self.sb(st, "of", [128, 512], F32) for _ in range(3)]
            ob = [self.sb(st, "ob", [128, 512], BF16) for _ in range(3)]
            with ExitStack() as st2:
                for tb in range(cfg.NBLK):
                    t0 = tb * 512
                    kb.load(xT[:], xsrc[b].rearrange("(c p) t -> p c t", p=128)[:, :, t0:t0 + 512], xT)
                    if tb == 0:
                        self._sq = self.sb(st2, "sq", [128, 8, 512], F32)
                        self._rstd = self.sb(st2, "rstd", [128, 512], F32)
                    self.rms_core(xT, l * 16, hT[tb])
            groups = []
            c = 0
            i = 0
            while i < len(ents):
                c0 = ents[i][0]
                j = i
                while j < len(ents) and ents[j][0] + ents[j][1] - c0 <= 512 and (j == i or ents[j][0] == ents[j - 1][0] + ents[j - 1][1]):
                    j += 1
                groups.append(('fm', c0, ents[j - 1][0] + ents[j - 1][1] - c0, ents[i:j]))
                i = j
            for tm in tms:
                groups.append(('tm', tm[0], tm[1], tm))
            oi = 0
            for gi, (kind, c0, wdt, payload) in enumerate(groups):
                w = wt[gi % 2]
                kb.load(w[:, :, 0:wdt], wsrc[:, :, c0:c0 + wdt], w)
                for tb in range(cfg.NBLK):
                    t0 = tb * 512
                    h = hT[tb]
                    if kind == 'fm':
                        for (ec0, M, dest, dt, func) in payload:
                            ps = self.nps()
                            o0 = ec0 - c0
                            for kc in range(8):
                                kb.op('pe', lambda E: E.matmul(ps[0:M, :], lhsT=w[:, kc, o0:o0 + M], rhs=h[:, kc, :], start=(kc == 0), stop=(kc == 7)),
                                      r=(w, h), w=(ps,))
                            o = (of if dt == F32 else ob)[oi % 3]
                            oi += 1
                            kb.op('act', lambda E: E.activation(out=o[0:M, :], in_=ps[0:M, :], func=func), r=(ps,), w=(o,))
                            kb.store(dest(t0), o[0:M, :], o, q='pool' if oi % 2 else 'sp')
                    else:
                        (tc0, ncols, dest, dt) = payload
                        for j in range(4):
                            ps = self.nps()
                            for kc in range(8):
                                kb.op('pe', lambda E: E.matmul(ps[:, 0:ncols], lhsT=h[:, kc, 128 * j:128 * j + 128], rhs=w[:, kc, 0:ncols], start=(kc == 0), stop=(kc == 7)),
                                      r=(w, h), w=(ps,))
                            o = (of if dt == F32 else ob)[oi % 3]
                            oi += 1
                            kb.op('act', lambda E: E.activation(out=o[:, 0:ncols], in_=ps[:, 0:ncols], func=AF.Copy), r=(ps,), w=(o,))
                            kb.store(dest(t0 + 128 * j), o[:, 0:ncols], o, q='pool' if oi % 2 else 'sp')

    def rms_core(self, xT, gcol0, o):
        kb = self.kb
        sq, rstd = self._sq, self._rstd
        kb.op('act', lambda E: E.activation(out=sq[:], in_=xT[:], func=AF.Square), r=(xT,), w=(sq,))
        ps = self.nps()
        for kc in range(8):
            kb.op('pe', lambda E: E.matmul(ps[:], lhsT=self.onesf, rhs=sq[:, kc, :], start=(kc == 0), stop=(kc == 7)), r=(sq, self.cf), w=(ps,))
        kb.op('act', lambda E: E.activation(out=rstd[:], in_=ps[:], func=AF.Sqrt, bias=self.epsb, scale=1.0 / D), r=(ps, self.cf), w=(rstd,))
        kb.op('dve', lambda E: E.reciprocal(out=rstd[:], in_=rstd[:]), r=(rstd,), w=(rstd,))
        for kc in range(8):
            kb.op('dve', lambda E: E.scalar_tensor_tensor(out=o[:, kc, :], in0=xT[:, kc, :], scalar=self.gv[:, gcol0 + kc:gcol0 + kc + 1],
                                                          in1=rstd[:], op0=ALU.mult, op1=ALU.mult), r=(xT, rstd, self.gv), w=(o,))

    def phase_M(self, l, b):
        kb, cfg = self.kb, self.cfg
        T, NL = cfg.T, cfg.NL
        last = (l == NL - 1)
        xsrc = self.x_in if l == 0 else self.xr
        with ExitStack() as st:
            wbr = self.sb(st, "wbr", [128, 16, D], BF16)
            wo = self.sb(st, "wo", [128, 8, D], BF16)
            kb.load(wbr[:], self.wb_br[l].rearrange("n (c p) o -> p (n c) o", p=128), wbr)
            kb.load(wo[:], self.wb_out[l].rearrange("(c p) o -> p c o", p=128), wo)
            w13 = [self.sb(st, "w13", [128, 8, 256], BF16) for _ in range(4)]
            w2t = [self.sb(st, "w2t", [128, 22, 256], BF16) for _ in range(2)]
            xT = self.sb(st, "xTm", [128, 8, 512], F32)
            mg = self.sb(st, "mg", [128, 8, 512], F32)
            mT = self.sb(st, "mT", [128, 8, 512], BF16)
            h2 = self.sb(st, "h2", [128, 8, 512], BF16)
            gT = self.sb(st, "gT", [128, 22, 512], BF16)
            yb = [self.sb(st, "yb", [128, 4, 512], BF16) for _ in range(2)]
            gsb = [self.sb(st, "gsb", [128, 8, 512], BF16) for _ in range(2)]
            tmp = [self.sb(st, "tmpm", [128, 512], F32) for _ in range(2)]
            s1 = [self.sb(st, "s1", [128, 512], BF16) for _ in range(2)]
            self._sq = mg
            self._rstd = self.sb(st, "rstd", [128, 512], F32)
            w1s = self.wb_1[l].rearrange("(c p) n -> p c n", p=128)
            w3s = self.wb_3[l].rearrange("(c p) n -> p c n", p=128)
            w2s = self.wb_2[l].rearrange("(c p) n -> p c n", p=128)
            ti = 0
            for tb in range(cfg.NBLK):
                t0 = tb * 512
                kb.load(xT[:], xsrc[b].rearrange("(c p) t -> p c t", p=128)[:, :, t0:t0 + 512], xT)
                for n in range(4):
                    y, g = yb[n % 2], gsb[n % 2]
                    kb.load(y[:], self.s_y4[b, n].rearrange("(c p) t -> p c t", p=128)[:, :, t0:t0 + 512], y)
                    kb.load(g[:], self.s_gs[b, n * 1024:(n + 1) * 1024].rearrange("(c p) t -> p c t", p=128)[:, :, t0:t0 + 512], g)
                    for oc in range(8):
                        ps = self.nps()
                        for kc in range(4):
                            kb.op('pe', lambda E: E.matmul(ps[:], lhsT=wbr[:, n * 4 + kc, oc * 128:oc * 128 + 128], rhs=y[:, kc, :], start=(kc == 0), stop=(kc == 3)),
                                  r=(wbr, y), w=(ps,))
                        if n == 0:
                            kb.op('dve', lambda E: E.tensor_tensor(out=mg[:, oc, :], in0=ps[:], in1=g[:, oc, :], op=ALU.mult), r=(ps, g), w=(mg,))
                        else:
                            tt = tmp[ti % 2]
                            ti += 1
                            kb.op('dve', lambda E: E.tensor_tensor(out=tt[:], in0=ps[:], in1=g[:, oc, :], op=ALU.mult), r=(ps, g), w=(tt,))
                            if n < 3:
                                kb.op('pool', lambda E: E.tensor_tensor(out=mg[:, oc, :], in0=mg[:, oc, :], in1=tt[:], op=ALU.add), r=(tt, mg), w=(mg,))
                            else:
                                kb.op('pool', lambda E: E.tensor_tensor(out=mT[:, oc, :], in0=mg[:, oc, :], in1=tt[:], op=ALU.add), r=(tt, mg), w=(mT,))
                for oc in range(8):
                    ps = self.nps()
                    for kc in range(8):
                        kb.op('pe', lambda E: E.matmul(ps[:], lhsT=wo[:, kc, oc * 128:oc * 128 + 128], rhs=mT[:, kc, :], start=(kc == 0), stop=(kc == 7)),
                              r=(wo, mT), w=(ps,))
                    kb.op('dve', lambda E: E.tensor_tensor(out=xT[:, oc, :], in0=ps[:], in1=xT[:, oc, :], op=ALU.add), r=(ps, xT), w=(xT,))
                self.rms_core(xT, l * 16 + 8, h2)
                ci = 0
                for c0 in range(0, FH, 256):
                    wd = min(256, FH - c0)
                    wa, wb_ = w13[(2 * ci) % 4], w13[(2 * ci + 1) % 4]
                    ci += 1
                    kb.load(wa[:, :, 0:wd], w1s[:, :, c0:c0 + wd], wa)
                    kb.load(wb_[:, :, 0:wd], w3s[:, :, c0:c0 + wd], wb_)
                    for j in range(wd // 128):
                        p1, p3 = self.nps(), self.nps()
                        for kc in range(8):
                            kb.op('pe', lambda E: E.matmul(p1[:], lhsT=wa[:, kc, j * 128:j * 128 + 128], rhs=h2[:, kc, :], start=(kc == 0), stop=(kc == 7)), r=(wa, h2), w=(p1,))
                        for kc in range(8):
                            kb.op('pe', lambda E: E.matmul(p3[:], lhsT=wb_[:, kc, j * 128:j * 128 + 128], rhs=h2[:, kc, :], start=(kc == 0), stop=(kc == 7)), r=(wb_, h2), w=(p3,))
                        s = s1[ti % 2]
                        ti += 1
                        kb.op('act', lambda E: E.activation(out=s[:], in_=p1[:], func=AF.Silu), r=(p1,), w=(s,))
                        hj = (c0 // 128) + j
                        kb.op('dve', lambda E: E.tensor_tensor(out=gT[:, hj, :], in0=p3[:], in1=s[:], op=ALU.mult), r=(p3, s), w=(gT,))
                for q4 in range(4):
                    w2c = w2t[q4 % 2]
                    kb.load(w2c[:], w2s[:, :, q4 * 256:q4 * 256 + 256], w2c)
                    for j in range(2):
                        oc = q4 * 2 + j
                        ps = self.nps()
                        for hj in range(22):
                            kb.op('pe', lambda E: E.matmul(ps[:], lhsT=w2c[:, hj, j * 128:j * 128 + 128], rhs=gT[:, hj, :], start=(hj == 0), stop=(hj == 21)), r=(w2c, gT), w=(ps,))
                        kb.op('dve', lambda E: E.tensor_tensor(out=xT[:, oc, :], in0=ps[:], in1=xT[:, oc, :], op=ALU.add), r=(ps, xT), w=(xT,))
                if not last:
                    kb.store(self.xr[b].rearrange("(c p) t -> p c t", p=128)[:, :, t0:t0 + 512], xT[:], xT, q='sp')
                else:
                    self.rms_core(xT, NL * 16, mg)
                    kb.store(self.out[b].rearrange("(c p) t -> p c t", p=128)[:, :, t0:t0 + 512], mg[:], mg, q='sp')

    def phase_A(self, l, b):
        kb, cfg, nc = self.kb, self.cfg, self.nc
        T = cfg.T
        NCH = T // 128
        P0 = PV_A
        identf = self.cf[:, 128:256]
        Uf = self.cf[:, 256:384]
        ones1 = self.cf[:, 0:1]
        with ExitStack() as st:
            pv = self.sb(st, "pv", [128, NPV], F32)
            kb.load(pv[:], self.pvec[l], pv)
            cv = self.sb(st, "cv", [128, 5, T], BF16)
            cvbc = self.sb(st, "cvbc", [64, 4, T], BF16)
            xsT = self.sb(st, "xsT", [128, NCH, 512], BF16)
            BT = self.sb(st, "BT", [128, NCH, 128], BF16)
            identb = self.sb(st, "identb", [128, 128], BF16)
            Ub = self.sb(st, "Ub", [128, 128], F32)
            kb.op('dve', lambda E: E.tensor_copy(out=identb[:], in_=identf), r=(self.cf,), w=(identb,))
            with ExitStack() as s2:
                xin = [self.sb(s2, "xin", [128, T + 3], F32) for _ in range(2)]
                acc = [self.sb(s2, "acc", [128, T], F32) for _ in range(2)]
                for i in range(9):
                    xi, ac = xin[i % 2], acc[i % 2]
                    if i < 4:
                        r0, np_, dst = 128 * i, 128, cv[:, i, :]
                    elif i < 8:
                        r0, np_, dst = 512 + 64 * (i - 4), 64, cvbc[:, i - 4, :]
                    else:
                        r0, np_, dst = 512, 128, cv[:, 4, :]
                    kb.op('pool', lambda E: E.memset(xi[:, 0:3], 0.0), r=(), w=(xi,))
                    kb.load(xi[0:np_, 3:3 + T], self.s_xbc[b, r0:r0 + np_, :], xi)
                    wc = lambda j: pv[0:np_, P0 + i * 4 + j:P0 + i * 4 + j + 1]
                    kb.op('dve', lambda E: E.tensor_scalar(out=ac[0:np_, :], in0=xi[0:np_, 3:3 + T], scalar1=wc(3), scalar2=pv[0:np_, P0 + 36 + i:P0 + 37 + i], op0=ALU.mult, op1=ALU.add),
                          r=(xi, pv), w=(ac,))
                    for j in range(3):
                        kb.op('dve', lambda E: E.scalar_tensor_tensor(out=ac[0:np_, :], in0=xi[0:np_, j:j + T], scalar=wc(j), in1=ac[0:np_, :], op0=ALU.mult, op1=ALU.add),
                              r=(xi, pv, ac), w=(ac,))
                    kb.op('act', lambda E: E.activation(out=dst, in_=ac[0:np_, :], func=AF.Silu), r=(ac,), w=(cv, cvbc))
            kb.barrier()
            if cfg.stop == 1:
                return
            evi = 0
            for c in range(NCH):
                for half in range(1 if cfg.stop == 21 else 2):
                    ps = self.psT[half]
                    psb = ps[:, 0:512]
                    if half == 0:
                        for k in range(3):
                            kb.op('pe', lambda E: E.transpose(out=psb[:, k * 128:(k + 1) * 128], in_=cv[:, k, c * 128:(c + 1) * 128], identity=identb[:]),
                                  r=(cv, identb), w=(ps,))
                    else:
                        kb.op('pe', lambda E: E.transpose(out=psb[:, 0:128], in_=cv[:, 3, c * 128:(c + 1) * 128], identity=identb[:]), r=(cv, identb), w=(ps,))
                        kb.op('pe', lambda E: E.transpose(out=psb[:, 128:256], in_=cv[:, 4, c * 128:(c + 1) * 128], identity=identb[:]), r=(cv, identb), w=(ps,))
                    e = 'act' if evi % 2 else 'dve'
                    evi += 1
                    if half == 0:
                        if e == 'act':
                            kb.op('act', lambda E: E.activation(out=xsT[:, c, 0:384], in_=psb[:, 0:384], func=AF.Copy), r=(ps,), w=(xsT,))
                        else:
                            kb.op('dve', lambda E: E.tensor_copy(out=xsT[:, c, 0:384], in_=psb[:, 0:384]), r=(ps,), w=(xsT,))
                    else:
                        kb.op('dve', lambda E: E.tensor_copy(out=xsT[:, c, 384:512], in_=psb[:, 0:128]), r=(ps,), w=(xsT,))
                        if True:
                            kb.op('dve', lambda E: E.tensor_copy(out=BT[:, c, :], in_=psb[:, 128:256]), r=(ps,), w=(BT,))
            if cfg.stop in (2, 21, 22):
                return
            W8 = NCH * 8
            dtr = self.sb(st, "dtr", [128, NCH, 8], F32)
            dtt = self.sb(st, "dtt", [128, NCH, 8], F32)
            da = self.sb(st, "da", [128, NCH, 8], F32)
            acs = self.sb(st, "acs", [128, NCH, 8], F32)
            dec = self.sb(st, "dec", [128, NCH, 8], F32)
            dte = self.sb(st, "dte", [128, NCH, 8], F32)
            t8 = self.sb(st, "t8", [128, NCH, 8], F32)
            aneg = self.sb(st, "aneg", [128, 8], F32)
            kb.load(dtr[:], self.s_dt[b].rearrange("(c p) h -> p c h", p=128), dtr)
            bias_b = pv[:, P0 + 48:P0 + 56].unsqueeze(1).to_broadcast([128, NCH, 8])
            kb.op('dve', lambda E: E.tensor_tensor(out=dtr[:], in0=dtr[:], in1=bias_b, op=ALU.add), r=(dtr, pv), w=(dtr,))
            kb.op('dve', lambda E: E.scalar_tensor_tensor(out=t8[:], in0=dtr[:], scalar=-1.0, in1=dtr[:], op0=ALU.mult, op1=ALU.max), r=(dtr,), w=(t8,))
            kb.op('act', lambda E: E.activation(out=t8[:], in_=t8[:], func=AF.Exp, scale=-1.0), r=(t8,), w=(t8,))
            kb.op('act', lambda E: E.activation(out=t8[:], in_=t8[:], func=AF.Ln, bias=ones1, scale=1.0), r=(t8, self.cf), w=(t8,))
            kb.op('dve', lambda E: E.scalar_tensor_tensor(out=dtt[:], in0=dtr[:], scalar=0.0, in1=t8[:], op0=ALU.max, op1=ALU.add), r=(dtr, t8), w=(dtt,))
            kb.op('act', lambda E: E.activation(out=aneg[:], in_=pv[:, P0 + 56:P0 + 64], func=AF.Exp), r=(pv,), w=(aneg,))
            kb.op('dve', lambda E: E.scalar_tensor_tensor(out=da[:], in0=dtt[:], scalar=-1.0, in1=aneg[:].unsqueeze(1).to_broadcast([128, NCH, 8]), op0=ALU.mult, op1=ALU.mult),
                  r=(dtt, aneg), w=(da,))
            kb.op('dve', lambda E: E.tensor_copy(out=Ub[:], in_=Uf), r=(self.cf,), w=(Ub,))
            daf = da[:].rearrange("p c h -> p (c h)")
            p1, p2 = self.nps(), self.nps()
            kb.op('pe', lambda E: E.matmul(p1[:, 0:W8], lhsT=Uf, rhs=daf, start=True, stop=True), r=(self.cf, da), w=(p1,))
            kb.op('pe', lambda E: E.matmul(p2[:, 0:W8], lhsT=self.onesf, rhs=daf, start=True, stop=True), r=(self.cf, da), w=(p2,))
            kb.op('dve', lambda E: E.tensor_copy(out=acs[:].rearrange("p c h -> p (c h)"), in_=p1[:, 0:W8]), r=(p1,), w=(acs,))
            kb.op('act', lambda E: E.activation(out=dec[:].rearrange("p c h -> p (c h)"), in_=p2[:, 0:W8], func=AF.Exp), r=(p2,), w=(dec,))
            kb.op('dve', lambda E: E.tensor_tensor(out=dte[:].rearrange("p c h -> p (c h)"), in0=p2[:, 0:W8], in1=acs[:].rearrange("p c h -> p (c h)"), op=ALU.subtract),
                  r=(p2, acs), w=(dte,))
            kb.op('act', lambda E: E.activation(out=dte[:], in_=dte[:], func=AF.Exp), r=(dte,), w=(dte,))
            kb.op('dve', lambda E: E.tensor_tensor(out=dte[:], in0=dte[:], in1=dtt[:], op=ALU.mult), r=(dte, dtt), w=(dte,))
            if cfg.stop == 3:
                return
            Dm = self.sb(st, "Dm", [128, 8, 128], BF16)
            for h in range(8):
                kb.op('dve', lambda E: E.tensor_scalar(out=Dm[:, h, :], in0=identf, scalar1=pv[:, P0 + 64 + h:P0 + 65 + h], scalar2=None, op0=ALU.mult),
                      r=(self.cf, pv), w=(Dm,))
            S32 = self.sb(st, "S32", [64, 8, 64], F32)
            Sbf = self.sb(st, "Sbf", [64, 8, 64], BF16)
            kb.op('pool', lambda E: E.memset(S32[:], 0.0), w=(S32,))
            kb.op('pool', lambda E: E.memset(Sbf[:], 0.0), w=(Sbf,))
            DA4 = [self.sb(st, "DA4", [128, 4, 128], F32) for _ in range(2)]
            E1 = [self.sb(st, "E1", [64, 4, 128], F32) for _ in range(2)]
            d2 = [self.sb(st, "d2", [128, 4, 128], F32) for _ in range(2)]
            G4 = [self.sb(st, "G4", [128, 4, 128], BF16) for _ in range(2)]
            CE4 = [self.sb(st, "CE4", [64, 4, 128], BF16) for _ in range(2)]
            Bs4 = [self.sb(st, "Bs4", [128, 4, 64], BF16) for _ in range(2)]
            zt = [self.sb(st, "zt", [64, 8, 128], F32) for _ in range(2)]
            yz = [self.sb(st, "yz", [64, 8, 128], F32) for _ in range(2)]
            ysq = [self.sb(st, "ysq", [64, 8, 128], F32) for _ in range(2)]
            rs = [self.sb(st, "rs", [64, 2, 128], F32) for _ in range(2)]
            yo = [self.sb(st, "yo", [64, 8, 128], BF16) for _ in range(2)]
            ydst = self.s_y4[b, 0].rearrange("(h p) t -> p h t", p=64)
            it = 0
            for c in range(NCH):
                t0 = c * 128
                z_, yz_, ysq_, rs_, yo_ = zt[c % 2], yz[c % 2], ysq[c % 2], rs[c % 2], yo[c % 2]
                kb.load(z_[:], self.s_z[b, :, :, t0:t0 + 128], z_)
                kb.op('act', lambda E: E.activation(out=z_[:], in_=z_[:], func=AF.Silu), r=(z_,), w=(z_,))
                for g in range(2):
                    k = it % 2
                    it += 1
                    hs = slice(4 * g, 4 * g + 4)
                    pA, pB, pY, pC = self.nps(), self.nps(), self.nps(), self.nps()
                    Bfm = cvbc[:, g, t0:t0 + 128]
                    Cfm = cvbc[:, 2 + g, t0:t0 + 128]
                    kb.op('pe', lambda E: E.matmul(pA[:, 0:128], lhsT=Bfm, rhs=Cfm, start=True, stop=True), r=(cvbc,), w=(pA,))
                    if cfg.stop == 41:
                        continue
                    kb.op('dve', lambda E: E.tensor_tensor(out=DA4[k][:], in0=self.cf[:, 0:128].unsqueeze(1).to_broadcast([128, 4, 128]),
                                                            in1=da[:, c, hs].unsqueeze(2).to_broadcast([128, 4, 128]), op=ALU.mult), r=(self.cf, da), w=(DA4[k],))
                    for hh in range(4):
                        kb.op('pe', lambda E: E.matmul(pB[:, hh * 128:hh * 128 + 128], lhsT=DA4[k][:, hh, :], rhs=Ub[:], start=True, stop=True), r=(DA4[k], Ub), w=(pB,))
                    kb.op('act', lambda E: E.activation(out=E1[k][:].rearrange("p a b -> p (a b)"), in_=pB[0:64, :], func=AF.Exp), r=(pB,), w=(E1[k],))
                    if cfg.stop == 42:
                        continue
                    kb.op('dve', lambda E: E.tensor_tensor(out=d2[k][:], in0=pB[:].rearrange("p (a b) -> p a b", a=4),
                                                           in1=acs[:, c, hs].unsqueeze(2).to_broadcast([128, 4, 128]), op=ALU.subtract), r=(pB, acs), w=(d2[k],))
                    if cfg.stop == 44:
                        continue
                    kb.op('dve', lambda E: E.tensor_scalar(out=d2[k][:], in0=d2[k][:], scalar1=0.0, scalar2=None, op0=ALU.min), r=(d2[k],), w=(d2[k],))
                    if cfg.stop == 45:
                        continue
                    kb.op('act', lambda E: E.activation(out=d2[k][:], in_=d2[k][:], func=AF.Exp), r=(d2[k],), w=(d2[k],))
                    if cfg.stop == 46:
                        continue
                    kb.op('dve', lambda E: E.tensor_tensor(out=d2[k][:], in0=d2[k][:], in1=Ub[:].unsqueeze(1).to_broadcast([128, 4, 128]), op=ALU.mult), r=(d2[k], Ub), w=(d2[k],))
                    kb.op('dve', lambda E: E.tensor_tensor(out=d2[k][:], in0=d2[k][:], in1=dtt[:, c, hs].unsqueeze(2).to_broadcast([128, 4, 128]), op=ALU.mult), r=(d2[k], dtt), w=(d2[k],))
                    if cfg.stop == 43:
                        continue
                    kb.op('dve', lambda E: E.tensor_tensor(out=G4[k][:], in0=d2[k][:], in1=pA[:, 0:128].unsqueeze(1).to_broadcast([128, 4, 128]), op=ALU.mult), r=(d2[k], pA), w=(G4[k],))
                    kb.op('dve', lambda E: E.tensor_tensor(out=CE4[k][:], in0=E1[k][:], in1=Cfm.unsqueeze(1).to_broadcast([64, 4, 128]), op=ALU.mult), r=(E1[k], cvbc), w=(CE4[k],))
                    kb.op('dve', lambda E: E.tensor_tensor(out=Bs4[k][:], in0=BT[:, c, 64 * g:64 * g + 64].unsqueeze(1).to_broadcast([128, 4, 64]),
                                                            in1=dte[:, c, hs].unsqueeze(2).to_broadcast([128, 4, 64]), op=ALU.mult), r=(BT, dte), w=(Bs4[k],))
                    if cfg.stop == 4:
                        continue
                    for hh in range(4):
                        h = 4 * g + hh
                        xh = xsT[:, c, h * 64:h * 64 + 64]
                        kb.op('pe', lambda E: E.matmul(pY[0:64, hh * 128:hh * 128 + 128], lhsT=xh, rhs=G4[k][:, hh, :], start=True, stop=False), r=(xsT, G4[k]), w=(pY,))
                        kb.op('pe', lambda E: E.matmul(pY[0:64, hh * 128:hh * 128 + 128], lhsT=xh, rhs=Dm[:, h, :], start=False, stop=False), r=(xsT, Dm), w=(pY,))
                        kb.op('pe', lambda E: E.matmul(pY[0:64, hh * 128:hh * 128 + 128], lhsT=Sbf[:, h, :], rhs=CE4[k][:, hh, :], start=False, stop=True), r=(Sbf, CE4[k]), w=(pY,))
                        kb.op('pe', lambda E: E.matmul(pC[0:64, hh * 64:hh * 64 + 64], lhsT=Bs4[k][:, hh, :], rhs=xh, start=True, stop=True), r=(Bs4[k], xsT), w=(pC,))
                    kb.op('dve', lambda E: E.tensor_tensor(out=S32[:, hs, :], in0=S32[:, hs, :], in1=dec[0:64, c, hs].unsqueeze(2).to_broadcast([64, 4, 64]), op=ALU.mult), r=(S32, dec), w=(S32,))
                    kb.op('dve', lambda E: E.tensor_tensor(out=S32[:, hs, :], in0=S32[:, hs, :], in1=pC[0:64, 0:256].rearrange("p (a b) -> p a b", a=4), op=ALU.add), r=(S32, pC), w=(S32,))
                    kb.op('act', lambda E: E.activation(out=Sbf[:, hs, :], in_=S32[:, hs, :], func=AF.Copy), r=(S32,), w=(Sbf,))
                    if cfg.stop == 5:
                        continue
                    kb.op('dve', lambda E: E.tensor_tensor(out=yz_[:, hs, :], in0=pY[0:64, :].rearrange("p (a b) -> p a b", a=4), in1=z_[:, hs, :], op=ALU.mult), r=(pY, z_), w=(yz_,))
                    kb.op('act', lambda E: E.activation(out=ysq_[:, hs, :], in_=yz_[:, hs, :], func=AF.Square), r=(yz_,), w=(ysq_,))
                    pN = self.nps()
                    for hh in range(4):
                        kb.op('pe', lambda E: E.matmul(pN[0:64, 0:128], lhsT=self.cf[0:64, 0:64], rhs=ysq_[:, 4 * g + hh, :], start=(hh == 0), stop=(hh == 3)), r=(self.cf, ysq_), w=(pN,))
                    kb.op('act', lambda E: E.activation(out=rs_[:, g, :], in_=pN[0:64, 0:128], func=AF.Sqrt, bias=self.cf[0:64, 384:385], scale=1.0 / 256), r=(pN, self.cf), w=(rs_,))
                    kb.op('dve', lambda E: E.reciprocal(out=rs_[:, g, :], in_=rs_[:, g, :]), r=(rs_,), w=(rs_,))
                    kb.op('dve', lambda E: E.tensor_tensor(out=yz_[:, hs, :], in0=yz_[:, hs, :], in1=rs_[:, g, :].unsqueeze(1).to_broadcast([64, 4, 128]), op=ALU.mult), r=(yz_, rs_), w=(yz_,))
                    kb.op('dve', lambda E: E.tensor_tensor(out=yo_[:, hs, :], in0=yz_[:, hs, :], in1=pv[0:64, P0 + 72 + 4 * g:P0 + 76 + 4 * g].unsqueeze(2).to_broadcast([64, 4, 128]), op=ALU.mult),
                          r=(yz_, pv), w=(yo_,))
                if cfg.stop in (4, 5, 41, 42, 43, 44, 45, 46):
                    continue
                kb.store(ydst[:, :, t0:t0 + 128], yo_[:], yo_, q='sp')

    def phase_D(self, l, b):
        kb, cfg, nc = self.kb, self.cfg, self.nc
        T = cfg.T
        NB = T // 512
        P0 = PV_D
        PI = float(np.pi)
        with ExitStack() as st:
            pv = self.sb(st, "pv", [128, NPV], F32)
            kb.load(pv[:], self.pvec[l], pv)
            lidx = self.sb(st, "lidx", [128, 512], F32)
            kb.load(lidx[:], self.cst_l[:, :], lidx)
            WB = self.sb(st, "WB", [128, 32, 128], F32)
            WC = self.sb(st, "WC", [128, 32, 128], F32)
            kb.load(WB[:], self.s5wb[l].rearrange("r i k m -> k (r i) m"), WB)
            kb.load(WC[:], self.s5wc[l].rearrange("r i k m -> k (r i) m"), WC)
            gw = self.sb(st, "gw", [128, 4, 512], F32)
            kb.load(gw[:], self.s5glu[l].rearrange("(c p) n -> p c n", p=128), gw)
            sm = self.sb(st, "sm", [128, 16, 16], F32)
            ARE, AIM, DT, RHO, TH, COS, SIN, CR, CI, C5, S5_, T1, T2, T3 = range(14)
            v = lambda k: sm[:, k, :]
            op = kb.op
            def dve(fn, r, w):
                op('dve', fn, r=r, w=w)
            ki = self.sb(st, "ki", [128, 512], mybir.dt.int32)
            kf = self.sb(st, "kf", [128, 512], F32)
            rr_ = self.sb(st, "rr", [128, 512], F32)

            def reduce_sin(dst, x, n, rk, add):
                a, kk, kff = rr_[:, 0:n], ki[:, 0:n], kf[:, 0:n]
                dve(lambda E: E.tensor_scalar(out=a, in0=x, scalar1=add, scalar2=None, op0=ALU.add), rk, (rr_,))
                dve(lambda E: E.tensor_scalar(out=kff, in0=a, scalar1=1.0 / (2 * PI), scalar2=None, op0=ALU.mult), (rr_,), (kf,))
                dve(lambda E: E.tensor_copy(out=kk, in_=kff), (kf,), (ki,))
                dve(lambda E: E.tensor_copy(out=kff, in_=kk), (ki,), (kf,))
                dve(lambda E: E.scalar_tensor_tensor(out=a, in0=kff, scalar=-2 * PI, in1=a, op0=ALU.mult, op1=ALU.add), (kf, rr_), (rr_,))
                dve(lambda E: E.tensor_scalar(out=kff, in0=a, scalar1=PI, scalar2=-2 * PI, op0=ALU.is_gt, op1=ALU.mult), (rr_,), (kf,))
                dve(lambda E: E.tensor_tensor(out=a, in0=a, in1=kff, op=ALU.add), (rr_, kf), (rr_,))
                dve(lambda E: E.tensor_scalar(out=kff, in0=a, scalar1=-PI, scalar2=2 * PI, op0=ALU.is_lt, op1=ALU.mult), (rr_,), (kf,))
                dve(lambda E: E.tensor_tensor(out=a, in0=a, in1=kff, op=ALU.add), (rr_, kf), (rr_,))
                op('act', lambda E: E.activation(out=dst, in_=a, func=AF.Sin), r=(rr_,), w=rk[-1:] if False else ())

            def sincos(dst_s, dst_c, src, tmp):
                reduce_sin(dst_s, src, 16, (sm,), 0.0)
                kb.record(('act', kb.cnt['act']), (), (sm,))
                reduce_sin(dst_c, src, 16, (sm,), PI / 2)
                kb.record(('act', kb.cnt['act']), (), (sm,))
            op('act', lambda E: E.activation(out=v(DT), in_=pv[:, P0 + 32:P0 + 48], func=AF.Exp), r=(pv,), w=(sm,))
            dve(lambda E: E.tensor_tensor(out=v(RHO), in0=pv[:, P0:P0 + 16], in1=v(DT), op=ALU.mult), (pv, sm), (sm,))
            op('act', lambda E: E.activation(out=v(RHO), in_=v(RHO), func=AF.Exp), r=(sm,), w=(sm,))
            dve(lambda E: E.tensor_tensor(out=v(TH), in0=pv[:, P0 + 16:P0 + 32], in1=v(DT), op=ALU.mult), (pv, sm), (sm,))
            sincos(v(SIN), v(COS), v(TH), v(T1))
            dve(lambda E: E.tensor_scalar(out=v(T2), in0=v(TH), scalar1=512.0, scalar2=None, op0=ALU.mult), (sm,), (sm,))
            sincos(v(S5_), v(C5), v(T2), v(T1))
            dve(lambda E: E.tensor_tensor(out=v(T1), in0=v(RHO), in1=v(COS), op=ALU.mult), (sm,), (sm,))
            dve(lambda E: E.tensor_scalar(out=v(T1), in0=v(T1), scalar1=-1.0, scalar2=None, op0=ALU.add), (sm,), (sm,))
            dve(lambda E: E.tensor_tensor(out=v(T2), in0=v(RHO), in1=v(SIN), op=ALU.mult), (sm,), (sm,))
            are, aim = pv[:, P0:P0 + 16], pv[:, P0 + 16:P0 + 32]
            dve(lambda E: E.tensor_tensor(out=v(T3), in0=are, in1=are, op=ALU.mult), (pv,), (sm,))
            dve(lambda E: E.tensor_tensor(out=v(CR), in0=aim, in1=aim, op=ALU.mult), (pv,), (sm,))
            dve(lambda E: E.tensor_tensor(out=v(T3), in0=v(T3), in1=v(CR), op=ALU.add), (sm,), (sm,))
            dve(lambda E: E.reciprocal(out=v(T3), in_=v(T3)), (sm,), (sm,))
            dve(lambda E: E.tensor_tensor(out=v(CR), in0=v(T1), in1=are, op=ALU.mult), (sm, pv), (sm,))
            dve(lambda E: E.tensor_tensor(out=v(CI), in0=v(T2), in1=aim, op=ALU.mult), (sm, pv), (sm,))
            dve(lambda E: E.tensor_tensor(out=v(CR), in0=v(CR), in1=v(CI), op=ALU.add), (sm,), (sm,))
            dve(lambda E: E.tensor_tensor(out=v(CR), in0=v(CR), in1=v(T3), op=ALU.mult), (sm,), (sm,))
            dve(lambda E: E.tensor_tensor(out=v(CI), in0=v(T2), in1=are, op=ALU.mult), (sm, pv), (sm,))
            dve(lambda E: E.tensor_tensor(out=v(T2), in0=v(T1), in1=aim, op=ALU.mult), (sm, pv), (sm,))
            dve(lambda E: E.tensor_tensor(out=v(CI), in0=v(CI), in1=v(T2), op=ALU.subtract), (sm,), (sm,))
            dve(lambda E: E.tensor_tensor(out=v(CI), in0=v(CI), in1=v(T3), op=ALU.mult), (sm,), (sm,))
            dve(lambda E: E.tensor_scalar(out=v(T1), in0=v(CI), scalar1=-1.0, scalar2=None, op0=ALU.mult), (sm,), (sm,))
            dve(lambda E: E.tensor_scalar(out=v(T2), in0=v(CR), scalar1=-1.0, scalar2=None, op0=ALU.mult), (sm,), (sm,))
            wtmp = self.sb(st, "wtmp", [128, 128], F32)
            WC2 = WC
            for i in range(16):
                cr, nci, ncr = sm[:, CR, i:i + 1], sm[:, T1, i:i + 1], sm[:, T2, i:i + 1]
                dve(lambda E: E.tensor_copy(out=wtmp[:], in_=WC[:, i, :]), (WC,), (wtmp,))
                dve(lambda E: E.tensor_scalar(out=WC[:, i, :], in0=wtmp[:], scalar1=cr, scalar2=None, op0=ALU.mult), (wtmp, sm), (WC,))
                dve(lambda E: E.scalar_tensor_tensor(out=WC[:, i, :], in0=WC[:, 16 + i, :], scalar=nci, in1=WC[:, i, :], op0=ALU.mult, op1=ALU.add), (WC, sm), (WC,))
                dve(lambda E: E.tensor_scalar(out=WC[:, 16 + i, :], in0=WC[:, 16 + i, :], scalar1=ncr, scalar2=None, op0=ALU.mult), (WC, sm), (WC,))
                dve(lambda E: E.scalar_tensor_tensor(out=WC[:, 16 + i, :], in0=wtmp[:], scalar=nci, in1=WC[:, 16 + i, :], op0=ALU.mult, op1=ALU.add), (wtmp, sm, WC), (WC,))
            tc_ = self.sb(st, "tcos", [128, 16, 512], F32)
            ts_ = self.sb(st, "tsin", [128, 16, 512], F32)
            tmpT = [self.sb(st, "tmpT", [128, 512], F32) for _ in range(2)]
            for i in range(16):
                for j, (dst, off) in enumerate(((ts_, 0.0), (tc_, PI / 2))):
                    tt = tmpT[j]
                    dve(lambda E: E.tensor_scalar(out=tt[:], in0=lidx[:], scalar1=sm[:, TH, i:i + 1], scalar2=None, op0=ALU.mult), (lidx, sm), (tt,))
                    reduce_sin(dst[:, i, :], tt[:], 512, (tt,), off)
                    kb.record(('act', kb.cnt['act']), (), (dst,))
            u = [self.sb(st, "u5", [128, 4, 512], F32) for _ in range(1)]
            br = [self.sb(st, "br", [128, 512], F32) for _ in range(2)]
            bi = [self.sb(st, "bi", [128, 512], F32) for _ in range(2)]
            w1 = [self.sb(st, "w1_", [128, 512], F32) for _ in range(2)]
            w2 = [self.sb(st, "w2_", [128, 512], F32) for _ in range(2)]
            zr = [self.sb(st, "zr", [128, 512], F32) for _ in range(2)]
            zi = [self.sb(st, "zi", [128, 512], F32) for _ in range(2)]
            xr = self.sb(st, "xr5", [128, 4, 512], F32)
            xi = self.sb(st, "xi5", [128, 4, 512], F32)
            car = self.sb(st, "car", [128, 16, 2], F32)
            ct = self.sb(st, "ct", [128, 4], F32)
            g5 = self.sb(st, "g5", [128, 4, 512], F32)
            y1 = [self.sb(st, "y1", [128, 512], F32) for _ in range(2)]
            q1 = [self.sb(st, "q1", [128, 512], F32) for _ in range(2)]
            yd = [self.sb(st, "yd", [128, 512], BF16) for _ in range(2)]
            op('pool', lambda E: E.memset(car[:], 0.0), w=(car,))
            n = 0
            for tb in range(NB):
                t0 = tb * 512
                uu = u[0]
                kb.load(uu[:], self.s_u5[b].rearrange("(c p) t -> p c t", p=128)[:, :, t0:t0 + 512], uu)
                for ot in range(4):
                    for ii in range(4):
                        i = 4 * ot + ii
                        k = n % 2
                        n += 1
                        pr, pi_ = self.nps(), self.nps()
                        op('pe', lambda E: E.matmul(pr[:], lhsT=WB[:, i, :], rhs=uu[:, ot, :], start=True, stop=True), r=(WB, uu), w=(pr,))
                        op('pe', lambda E: E.matmul(pi_[:], lhsT=WB[:, 16 + i, :], rhs=uu[:, ot, :], start=True, stop=True), r=(WB, uu), w=(pi_,))
                        op('act', lambda E: E.activation(out=br[k][:], in_=pr[:], func=AF.Copy), r=(pr,), w=(br[k],))
                        op('act', lambda E: E.activation(out=bi[k][:], in_=pi_[:], func=AF.Copy), r=(pi_,), w=(bi[k],))
                        cs, sn = tc_[:, i, :], ts_[:, i, :]
                        op('dve', lambda E: E.tensor_tensor(out=w1[k][:], in0=br[k][:], in1=cs, op=ALU.mult), r=(br[k], tc_), w=(w1[k],))
                        op('pool', lambda E: E.tensor_tensor(out=w2[k][:], in0=bi[k][:], in1=sn, op=ALU.mult), r=(bi[k], ts_), w=(w2[k],))
                        op('dve', lambda E: E.tensor_tensor(out=zr[k][:], in0=w1[k][:], in1=w2[k][:], op=ALU.add), r=(w1[k], w2[k]), w=(zr[k],))
                        op('pool', lambda E: E.tensor_tensor(out=w1[k][:], in0=bi[k][:], in1=cs, op=ALU.mult), r=(bi[k], tc_), w=(w1[k],))
                        op('dve', lambda E: E.tensor_tensor(out=w2[k][:], in0=br[k][:], in1=sn, op=ALU.mult), r=(br[k], ts_), w=(w2[k],))
                        op('pool', lambda E: E.tensor_tensor(out=zi[k][:], in0=w1[k][:], in1=w2[k][:], op=ALU.subtract), r=(w1[k], w2[k]), w=(zi[k],))
                        rb = sm[:, RHO, i:i + 1].to_broadcast([128, 512])
                        op('dve', lambda E: E.tensor_tensor_scan(out=zr[k][:], data0=rb, data1=zr[k][:], initial=car[:, i, 0:1], op0=ALU.mult, op1=ALU.add), r=(sm, zr[k], car), w=(zr[k],))
                        op('dve', lambda E: E.tensor_tensor_scan(out=zi[k][:], data0=rb, data1=zi[k][:], initial=car[:, i, 1:2], op0=ALU.mult, op1=ALU.add), r=(sm, zi[k], car), w=(zi[k],))
                        c5, s5 = sm[:, C5, i:i + 1], sm[:, S5_, i:i + 1]
                        zl, zli = zr[k][:, 511:512], zi[k][:, 511:512]
                        op('dve', lambda E: E.tensor_scalar(out=ct[:, 0:1], in0=zl, scalar1=c5, scalar2=None, op0=ALU.mult), r=(zr[k], sm), w=(ct,))
                        op('dve', lambda E: E.tensor_scalar(out=ct[:, 1:2], in0=zli, scalar1=s5, scalar2=None, op0=ALU.mult), r=(zi[k], sm), w=(ct,))
                        op('dve', lambda E: E.tensor_scalar(out=ct[:, 2:3], in0=zl, scalar1=s5, scalar2=None, op0=ALU.mult), r=(zr[k], sm), w=(ct,))
                        op('dve', lambda E: E.tensor_scalar(out=ct[:, 3:4], in0=zli, scalar1=c5, scalar2=None, op0=ALU.mult), r=(zi[k], sm), w=(ct,))
                        op('dve', lambda E: E.tensor_tensor(out=car[:, i, 0:1], in0=ct[:, 0:1], in1=ct[:, 1:2], op=ALU.subtract), r=(ct,), w=(car,))
                        op('dve', lambda E: E.tensor_tensor(out=car[:, i, 1:2], in0=ct[:, 2:3], in1=ct[:, 3:4], op=ALU.add), r=(ct,), w=(car,))
                        op('dve', lambda E: E.tensor_tensor(out=w1[k][:], in0=zr[k][:], in1=cs, op=ALU.mult), r=(zr[k], tc_), w=(w1[k],))
                        op('pool', lambda E: E.tensor_tensor(out=w2[k][:], in0=zi[k][:], in1=sn, op=ALU.mult), r=(zi[k], ts_), w=(w2[k],))
                        op('dve', lambda E: E.tensor_tensor(out=xr[:, ii, :], in0=w1[k][:], in1=w2[k][:], op=ALU.subtract), r=(w1[k], w2[k]), w=(xr,))
                        op('pool', lambda E: E.tensor_tensor(out=w1[k][:], in0=zr[k][:], in1=sn, op=ALU.mult), r=(zr[k], ts_), w=(w1[k],))
                        op('dve', lambda E: E.tensor_tensor(out=w2[k][:], in0=zi[k][:], in1=cs, op=ALU.mult), r=(zi[k], tc_), w=(w2[k],))
                        op('pool', lambda E: E.tensor_tensor(out=xi[:, ii, :], in0=w1[k][:], in1=w2[k][:], op=ALU.add), r=(w1[k], w2[k]), w=(xi,))
                    py = self.nps()
                    for ii in range(4):
                        i = 4 * ot + ii
                        op('pe', lambda E: E.matmul(py[:], lhsT=WC2[:, i, :], rhs=xr[:, ii, :], start=(ii == 0), stop=False), r=(WC2, xr), w=(py,))
                        op('pe', lambda E: E.matmul(py[:], lhsT=WC2[:, 16 + i, :], rhs=xi[:, ii, :], start=False, stop=(ii == 3)), r=(WC2, xi), w=(py,))
                    k = ot % 2
                    op('dve', lambda E: E.scalar_tensor_tensor(out=y1[k][:], in0=uu[:, ot, :], scalar=pv[:, P0 + 48 + ot:P0 + 49 + ot], in1=py[:], op0=ALU.mult, op1=ALU.add), r=(uu, pv, py), w=(y1[k],))
                    op('act', lambda E: E.activation(out=q1[k][:], in_=y1[k][:], func=AF.Square), r=(y1[k],), w=(q1[k],))
                    op('dve', lambda E: E.tensor_scalar(out=q1[k][:], in0=q1[k][:], scalar1=0.044715, scalar2=1.0, op0=ALU.mult, op1=ALU.add), r=(q1[k],), w=(q1[k],))
                    op('pool', lambda E: E.tensor_tensor(out=q1[k][:], in0=q1[k][:], in1=y1[k][:], op=ALU.mult), r=(q1[k], y1[k]), w=(q1[k],))
                    op('act', lambda E: E.activation(out=q1[k][:], in_=q1[k][:], func=AF.Sigmoid, scale=1.5957691216057308), r=(q1[k],), w=(q1[k],))
                    op('pool', lambda E: E.tensor_tensor(out=g5[:, ot, :], in0=q1[k][:], in1=y1[k][:], op=ALU.mult), r=(q1[k], y1[k]), w=(g5,))
                for oc in range(4):
                    pg = self.nps()
                    for kc in range(4):
                        op('pe', lambda E: E.matmul(pg[:], lhsT=gw[:, kc, oc * 128:oc * 128 + 128], rhs=g5[:, kc, :], start=(kc == 0), stop=(kc == 3)), r=(gw, g5), w=(pg,))
                    k = oc % 2
                    op('act', lambda E: E.activation(out=q1[k][:], in_=pg[:], func=AF.Sigmoid, bias=pv[:, P0 + 52 + oc:P0 + 53 + oc], scale=1.0), r=(pg, pv), w=(q1[k],))
                    op('dve', lambda E: E.tensor_tensor(out=yd[k][:], in0=q1[k][:], in1=g5[:, oc, :], op=ALU.mult), r=(q1[k], g5), w=(yd[k],))
                    kb.store(self.s_y4[b, 3, oc * 128:oc * 128 + 128, t0:t0 + 512], yd[k][:], yd[k], q='sp')

    def phase_B(self, l, b):
        kb, cfg, nc = self.kb, self.cfg, self.nc
        op = kb.op
        T = cfg.T
        NT = T // 128
        TOPK = min(256, T // 4)
        NEG = -1.0e30
        identf = self.cf[:, 128:256]
        with ExitStack() as st:
            Kf = self.sb(st, "Kf", [64, 8, T], BF16)
            Vt = self.sb(st, "Vt", [128, NT, 512], BF16)
            kif = self.sb(st, "kif", [32, T], BF16)
            kb.load(Kf[:], self.s_k[b], Kf)
            kb.load(Vt[:], self.s_v[b].rearrange("(n p) c -> p n c", p=128), Vt)
            kb.load(kif[:], self.s_ki[b], kif)
            identb = self.sb(st, "identb", [128, 128], BF16)
            onesb = self.sb(st, "onesb", [128, 64], BF16)
            op('dve', lambda E: E.tensor_copy(out=identb[:], in_=identf), r=(self.cf,), w=(identb,))
            op('dve', lambda E: E.tensor_copy(out=onesb[:], in_=self.cf[:, 0:64]), r=(self.cf,), w=(onesb,))
            EBM = self.sb(st, "EBM", [128, 4, 8, 128], BF16)
            with ExitStack() as s2:
                rb = self.sb(s2, "rb", [128, 256], F32)
                kb.load(rb[:], self.relb[:, :], rb)
                oh = self.sb(s2, "oh", [128, len(OH_LIST), 128], F32)
                kb.load(oh[:], self.cst_oh[:, :, :], oh)
                acc = [self.sb(s2, "bacc", [128, 128], F32) for _ in range(2)]
                n = 0
                for c in range(3):
                    js = [j for j, (cc, bk) in enumerate(OH_LIST) if cc == c]
                    for h in range(8):
                        a = acc[n % 2]
                        n += 1
                        for jj, j in enumerate(js):
                            bk = OH_LIST[j][1]
                            sc = rb[:, bk * 8 + h:bk * 8 + h + 1]
                            if jj == 0:
                                op('dve', lambda E: E.tensor_scalar(out=a[:], in0=oh[:, j, :], scalar1=sc, scalar2=None, op0=ALU.mult), r=(oh, rb), w=(a,))
                            else:
                                op('dve', lambda E: E.scalar_tensor_tensor(out=a[:], in0=oh[:, j, :], scalar=sc, in1=a[:], op0=ALU.mult, op1=ALU.add), r=(oh, rb, a), w=(a,))
                        op('act', lambda E: E.activation(out=EBM[:, c, h, :], in_=a[:], func=AF.Exp), r=(a,), w=(EBM,))
                for h in range(8):
                    a = acc[n % 2]
                    n += 1
                    op('dve', lambda E: E.tensor_scalar(out=a[:], in0=self.cf[:, 0:128], scalar1=rb[:, 15 * 8 + h:15 * 8 + h + 1], scalar2=None, op0=ALU.mult), r=(self.cf, rb), w=(a,))
                    op('act', lambda E: E.activation(out=EBM[:, 3, h, :], in_=a[:], func=AF.Exp), r=(a,), w=(EBM,))
            kb.barrier()
            score = self.sb(st, "score", [128, T], F32)
            work = self.sb(st, "work", [128, T], F32)
            ramp = self.sb(st, "ramp", [128, 512], F32)
            kb.load(ramp[:, 0:512], self.cst_l[:, :], ramp)
            op('dve', lambda E: E.tensor_scalar(out=ramp[:], in0=ramp[:], scalar1=-1.0e-7, scalar2=None, op0=ALU.mult), r=(ramp,), w=(ramp,))
            maskb = self.sb(st, "maskb", [128, T], BF16)
            maskT = self.sb(st, "maskT", [128, NT, 128], BF16)
            rl = [self.sb(st, "rl", [128, 512], F32) for _ in range(2)]
            m8 = self.sb(st, "m8", [128, 8], F32)
            thr = self.sb(st, "thr", [128, 1], F32)
            qf = [self.sb(st, "qf", [64, 8, 128], BF16) for _ in range(2)]
            qif = [self.sb(st, "qif", [32, 4, 128], BF16) for _ in range(2)]
            wif = [self.sb(st, "wif", [128, 4], F32) for _ in range(2)]
            ex = [self.sb(st, "ex", [128, 8, 128], BF16) for _ in range(2)]
            M2 = [self.sb(st, "M2", [128, 8, 128], BF16) for _ in range(2)]
            Pm = [self.sb(st, "Pm", [128, 8, 128], BF16) for _ in range(2)]
            rec = self.sb(st, "rec", [64, 1024], F32)
            accO = self.sb(st, "accO", [64, 1024], F32)
            accD = self.sb(st, "accD", [64, 1024], F32)
            yb = [self.sb(st, "ybB", [64, 8, 128], BF16) for _ in range(2)]
            ydst = self.s_y4[b, 1].rearrange("(h p) t -> p h t", p=64)
            pO = (self.ps[0], self.ps[1])
            pD = (self.ps[2], self.ps[3])
            pL = (self.ps[4], self.ps[5])
            it = 0
            for qt in range(NT):
                q0 = qt * 128
                S = q0 + 128
                q_, qi_, wi_ = qf[qt % 2], qif[qt % 2], wif[qt % 2]
                kb.load(q_[:], self.s_q[b, :, :, q0:q0 + 128], q_)
                kb.load(qi_[:], self.s_qi[b, :, :, q0:q0 + 128], qi_)
                kb.load(wi_[:], self.s_wi[b, q0:q0 + 128, :], wi_)
                for s0 in range(0, S, 512):
                    wd = min(512, S - s0)
                    for hh in range(4):
                        ps = pL[hh % 2]
                        r_ = rl[hh % 2]
                        op('pe', lambda E: E.matmul(ps[:, 0:wd], lhsT=qi_[:, hh, :], rhs=kif[:, s0:s0 + wd], start=True, stop=True), r=(qi_, kif), w=(ps,))
                        op('act', lambda E: E.activation(out=r_[:, 0:wd], in_=ps[:, 0:wd], func=AF.Relu), r=(ps,), w=(r_,))
                        if hh == 0:
                            op('dve', lambda E: E.scalar_tensor_tensor(out=score[:, s0:s0 + wd], in0=r_[:, 0:wd], scalar=wi_[:, 0:1], in1=ramp[:, 0:wd],
                                                                       op0=ALU.mult, op1=ALU.add), r=(r_, wi_, ramp), w=(score,))
                            if s0 > 0:
                                op('dve', lambda E: E.tensor_scalar(out=score[:, s0:s0 + wd], in0=score[:, s0:s0 + wd], scalar1=-1.0e-7 * s0, scalar2=None, op0=ALU.add), r=(score,), w=(score,))
                        else:
                            op('dve', lambda E: E.scalar_tensor_tensor(out=score[:, s0:s0 + wd], in0=r_[:, 0:wd], scalar=wi_[:, hh:hh + 1], in1=score[:, s0:s0 + wd],
                                                                       op0=ALU.mult, op1=ALU.add), r=(r_, wi_, score), w=(score,))
                op('pool', lambda E: E.memset(score[0:64, q0 + 64:q0 + 128], NEG), r=(), w=(score,))
                if cfg.stop == 61:
                    continue
                if S <= TOPK:
                    op('pool', lambda E: E.memset(thr[:], -1.0e29), w=(thr,))
                else:
                    nr = TOPK // 8
                    for r in range(nr):
                        src = score if r == 0 else work
                        op('dve', lambda E: E.max(out=m8[:], in_=src[:, 0:S]), r=(src,), w=(m8,))
                        if r < nr - 1:
                            op('dve', lambda E: E.match_replace(out=work[:, 0:S], in_to_replace=m8[:], in_values=src[:, 0:S], imm_value=NEG), r=(src, m8), w=(work,))
                    op('dve', lambda E: E.tensor_copy(out=thr[:], in_=m8[:, 7:8]), r=(m8,), w=(thr,))
                op('dve', lambda E: E.tensor_scalar(out=maskb[:, 0:S], in0=score[:, 0:S], scalar1=thr[:, 0:1], scalar2=None, op0=ALU.is_ge), r=(score, thr), w=(maskb,))
                if cfg.stop == 62:
                    continue
                for g0 in range(0, qt + 1, 8):
                    ng = min(8, qt + 1 - g0)
                    pt = self.psT[(g0 // 8) % 2]
                    for j in range(ng):
                        op('pe', lambda E: E.transpose(out=pt[:, j * 128:(j + 1) * 128], in_=maskb[:, (g0 + j) * 128:(g0 + j + 1) * 128], identity=identb[:]), r=(maskb, identb), w=(pt,))
                    op('dve', lambda E: E.tensor_copy(out=maskT[:, g0:g0 + ng, :].rearrange("p a b -> p (a b)"), in_=pt[:, 0:ng * 128]), r=(pt,), w=(maskT,))
                if cfg.stop == 63:
                    continue
                for kbk in range(qt + 1):
                    k0 = kbk * 128
                    cl = min(qt - kbk, 3)
                    k = it % 2
                    it += 1
                    for hg in range(2):
                        for hh in range(4):
                            h = hg * 4 + hh
                            op('pe', lambda E: E.matmul(pL[hg][:, hh * 128:hh * 128 + 128], lhsT=Kf[:, h, k0:k0 + 128], rhs=q_[:, h, :], start=True, stop=True), r=(Kf, q_), w=(pL[hg],))
                        op('act', lambda E: E.activation(out=ex[k][:, hg * 4:hg * 4 + 4, :].rearrange("p a b -> p (a b)"), in_=pL[hg][:], func=AF.Exp, scale=0.125), r=(pL[hg],), w=(ex[k],))
                    op('dve', lambda E: E.tensor_tensor(out=M2[k][:], in0=EBM[:, cl, :, :], in1=maskT[:, kbk, :].unsqueeze(1).to_broadcast([128, 8, 128]), op=ALU.mult), r=(EBM, maskT), w=(M2[k],))
                    op('dve', lambda E: E.tensor_tensor(out=Pm[k][:], in0=ex[k][:], in1=M2[k][:], op=ALU.mult), r=(ex[k], M2[k]), w=(Pm[k],))
                    first = (kbk == 0)
                    for h in range(8):
                        op('pe', lambda E: E.matmul(pO[h // 4][0:64, (h % 4) * 128:(h % 4) * 128 + 128], lhsT=Vt[:, kbk, h * 64:h * 64 + 64], rhs=Pm[k][:, h, :], start=True, stop=True),
                           r=(Vt, Pm[k]), w=(pO[h // 4],))
                    for hg in range(2):
                        op('pe', lambda E: E.matmul(pD[hg][0:64, :], lhsT=onesb[:], rhs=Pm[k][:, hg * 4:hg * 4 + 4, :].rearrange("p a b -> p (a b)"), start=True, stop=True),
                           r=(onesb, Pm[k]), w=(pD[hg],))
                    for hg in range(2):
                        sl = slice(hg * 512, hg * 512 + 512)
                        if first:
                            op('dve', lambda E: E.tensor_copy(out=accO[:, sl], in_=pO[hg][0:64, :]), r=(pO[hg],), w=(accO,))
                            op('dve', lambda E: E.tensor_copy(out=accD[:, sl], in_=pD[hg][0:64, :]), r=(pD[hg],), w=(accD,))
                        else:
                            op('dve', lambda E: E.tensor_tensor(out=accO[:, sl], in0=accO[:, sl], in1=pO[hg][0:64, :], op=ALU.add), r=(pO[hg], accO), w=(accO,))
                            op('dve', lambda E: E.tensor_tensor(out=accD[:, sl], in0=accD[:, sl], in1=pD[hg][0:64, :], op=ALU.add), r=(pD[hg], accD), w=(accD,))
                y_ = yb[qt % 2]
                op('dve', lambda E: E.reciprocal(out=rec[:], in_=accD[:]), r=(accD,), w=(rec,))
                op('dve', lambda E: E.tensor_tensor(out=y_[:].rearrange("p a b -> p (a b)"), in0=accO[:], in1=rec[:], op=ALU.mult), r=(accO, rec), w=(y_,))
                kb.store(ydst[:, :, q0:q0 + 128], y_[:], y_, q='sp')

    def phase_C1(self, l, b):
        kb, cfg = self.kb, self.cfg
        op = kb.op
        T = cfg.T
        P0 = PV_C
        with ExitStack() as st:
            pv = self.sb(st, "pv", [128, NPV], F32)
            kb.load(pv[:], self.pvec[l], pv)
            bd = self.sb(st, "bd", [128, 128], F32)
            kb.load(bd[:], self.cst_bd[:, :], bd)
            wa2 = self.sb(st, "wa2", [128, 512], F32)
            g2 = self.sb(st, "g2", [128, 512], F32)
            kb.load(wa2[0:64, :], self.rw_w2[l], wa2)
            kb.load(wa2[64:128, :], self.rw_a2[l], wa2)
            kb.load(g2[:], self.rw_g2[l], g2)
            om = self.sb(st, "om", [128, 20], F32)
            op('dve', lambda E: E.tensor_scalar(out=om[:, 0:14], in0=pv[:, P0:P0 + 14], scalar1=-1.0, scalar2=1.0, op0=ALU.mult, op1=ALU.add), r=(pv,), w=(om,))
            op('dve', lambda E: E.tensor_scalar(out=om[:, 14:18], in0=pv[:, P0 + 26:P0 + 30], scalar1=-1.0, scalar2=1.0, op0=ALU.mult, op1=ALU.add), r=(pv,), w=(om,))
            MU, W0, A0, KK_, KA, RK, LG, LB = P0, P0 + 14, P0 + 18, P0 + 22, P0 + 26, P0 + 30, P0 + 34, P0 + 38
            xin = [self.sb(st, "rxin", [128, 513], F32) for _ in range(2)]
            sh = self.sb(st, "sh", [128, 14, 512], F32)
            t1 = [self.sb(st, "rt1", [128, 512], F32) for _ in range(2)]
            t2 = [self.sb(st, "rt2", [128, 512], F32) for _ in range(2)]
            av = self.sb(st, "av", [128, 4, 512], F32)
            o5 = [self.sb(st, "o5", [128, 512], F32) for _ in range(3)]
            ones1 = self.cf[:, 0:1]
            oi = 0
            def out(dst, tile, t0, src):
                kb.store(dst[tile * 128:tile * 128 + 128, t0:t0 + 512], src[:], src, q='sp')
            for tb in range(T // 512):
                t0 = tb * 512
                for i in range(14):
                    xi = xin[i % 2]
                    if tb == 0:
                        op('pool', lambda E: E.memset(xi[:, 0:1], 0.0), w=(xi,))
                        kb.load(xi[:, 1:513], self.s_rw[b, i * 128:i * 128 + 128, 0:512], xi)
                    else:
                        kb.load(xi[:, 0:513], self.s_rw[b, i * 128:i * 128 + 128, t0 - 1:t0 + 512], xi)
                    tt = t1[i % 2]
                    op('dve', lambda E: E.tensor_scalar(out=tt[:], in0=xi[:, 0:512], scalar1=pv[:, MU + i:MU + i + 1], scalar2=None, op0=ALU.mult), r=(xi, pv), w=(tt,))
                    op('dve', lambda E: E.scalar_tensor_tensor(out=sh[:, i, :], in0=xi[:, 1:513], scalar=om[:, i:i + 1], in1=tt[:], op0=ALU.mult, op1=ALU.add), r=(xi, om, tt), w=(sh,))
                op('act', lambda E: E.activation(out=sh[0:64, 12, :], in_=sh[0:64, 12, :], func=AF.Tanh), r=(sh,), w=(sh,))
                op('act', lambda E: E.activation(out=sh[:, 13, :], in_=sh[:, 13, :], func=AF.Sigmoid), r=(sh,), w=(sh,))
                for c in range(4):
                    cs = slice(c * 128, c * 128 + 128)
                    r_, k_, v_ = sh[:, c, :], sh[:, 4 + c, :], sh[:, 8 + c, :]
                    pw, pa, pg = self.nps(), self.nps(), self.nps()
                    op('pe', lambda E: E.matmul(pw[:], lhsT=wa2[0:64, cs], rhs=sh[0:64, 12, :], start=True, stop=True), r=(wa2, sh), w=(pw,))
                    op('pe', lambda E: E.matmul(pa[:], lhsT=wa2[64:128, cs], rhs=sh[64:128, 12, :], start=True, stop=True), r=(wa2, sh), w=(pa,))
                    op('pe', lambda E: E.matmul(pg[:], lhsT=g2[:, cs], rhs=sh[:, 13, :], start=True, stop=True), r=(g2, sh), w=(pg,))
                    x, y = t1[0], t2[0]
                    op('dve', lambda E: E.tensor_scalar(out=x[:], in0=pw[:], scalar1=pv[:, W0 + c:W0 + c + 1], scalar2=None, op0=ALU.add), r=(pw, pv), w=(x,))
                    op('dve', lambda E: E.scalar_tensor_tensor(out=y[:], in0=x[:], scalar=-1.0, in1=x[:], op0=ALU.mult, op1=ALU.max), r=(x,), w=(y,))
                    op('act', lambda E: E.activation(out=y[:], in_=y[:], func=AF.Exp, scale=-1.0), r=(y,), w=(y,))
                    op('act', lambda E: E.activation(out=y[:], in_=y[:], func=AF.Ln, bias=ones1, scale=1.0), r=(y, self.cf), w=(y,))
                    op('dve', lambda E: E.tensor_scalar(out=x[:], in0=x[:], scalar1=-1.0, scalar2=0.0, op0=ALU.mult, op1=ALU.max), r=(x,), w=(x,))
                    op('dve', lambda E: E.tensor_tensor(out=x[:], in0=x[:], in1=y[:], op=ALU.add), r=(x, y), w=(x,))
                    op('dve', lambda E: E.tensor_scalar(out=x[:], in0=x[:], scalar1=-1.0, scalar2=-0.5, op0=ALU.mult, op1=ALU.add), r=(x,), w=(x,))
                    op('act', lambda E: E.activation(out=x[:], in_=x[:], func=AF.Exp), r=(x,), w=(x,))
                    o = o5[oi % 3]; oi += 1
                    op('act', lambda E: E.activation(out=o[:], in_=x[:], func=AF.Exp, scale=-1.0), r=(x,), w=(o,))
                    out(self.s_rv[b, 1], c, t0, o)
                    op('act', lambda E: E.activation(out=av[:, c, :], in_=pa[:], func=AF.Sigmoid, bias=pv[:, A0 + c:A0 + c + 1], scale=1.0), r=(pa, pv), w=(av,))
                    a_ = av[:, c, :]
                    x2, y2 = t1[1], t2[1]
                    op('dve', lambda E: E.tensor_scalar(out=x2[:], in0=k_, scalar1=pv[:, KK_ + c:KK_ + c + 1], scalar2=None, op0=ALU.mult), r=(sh, pv), w=(x2,))
                    op('act', lambda E: E.activation(out=y2[:], in_=x2[:], func=AF.Square), r=(x2,), w=(y2,))
                    pn = self.nps()
                    op('pe', lambda E: E.matmul(pn[:], lhsT=bd[:], rhs=y2[:], start=True, stop=True), r=(bd, y2), w=(pn,))
                    op('act', lambda E: E.activation(out=y2[:], in_=pn[:], func=AF.Sqrt), r=(pn,), w=(y2,))
                    op('dve', lambda E: E.tensor_scalar(out=y2[:], in0=y2[:], scalar1=1e-12, scalar2=None, op0=ALU.max), r=(y2,), w=(y2,))
                    op('dve', lambda E: E.reciprocal(out=y2[:], in_=y2[:]), r=(y2,), w=(y2,))
                    o = o5[oi % 3]; oi += 1
                    op('dve', lambda E: E.tensor_tensor(out=o[:], in0=x2[:], in1=y2[:], op=ALU.mult), r=(x2, y2), w=(o,))
                    out(self.s_rv[b, 3], c, t0, o)
                    o2 = o5[oi % 3]; oi += 1
                    op('pool', lambda E: E.tensor_tensor(out=o2[:], in0=o[:], in1=a_, op=ALU.mult), r=(o, av), w=(o2,))
                    out(self.s_rv[b, 4], c, t0, o2)
                    op('dve', lambda E: E.tensor_scalar(out=x2[:], in0=a_, scalar1=pv[:, KA + c:KA + c + 1], scalar2=om[:, 14 + c:15 + c], op0=ALU.mult, op1=ALU.add), r=(av, pv, om), w=(x2,))
                    o = o5[oi % 3]; oi += 1
                    op('dve', lambda E: E.tensor_tensor(out=o[:], in0=x2[:], in1=k_, op=ALU.mult), r=(x2, sh), w=(o,))
                    out(self.s_rv[b, 2], c, t0, o)
                    kb.store(self.s_rv[b, 0][c * 128:c * 128 + 128, t0:t0 + 512], sh[:, c, :], sh, q='sp')
                    kb.store(self.s_rx[b, 0][c * 128:c * 128 + 128, t0:t0 + 512], sh[:, 8 + c, :], sh, q='sp')
                    op('dve', lambda E: E.scalar_tensor_tensor(out=y2[:], in0=o[:], scalar=pv[:, RK + c:RK + c + 1], in1=r_, op0=ALU.mult, op1=ALU.mult), r=(o, pv, sh), w=(y2,))
                    pb = self.nps()
                    op('pe', lambda E: E.matmul(pb[:], lhsT=bd[:], rhs=y2[:], start=True, stop=True), r=(bd, y2), w=(pb,))
                    o = o5[oi % 3]; oi += 1
                    op('dve', lambda E: E.tensor_tensor(out=o[:], in0=pb[:], in1=v_, op=ALU.mult), r=(pb, sh), w=(o,))
                    out(self.s_rx[b, 2], c, t0, o)
                    o = o5[oi % 3]; oi += 1
                    op('act', lambda E: E.activation(out=o[:], in_=pg[:], func=AF.Copy), r=(pg,), w=(o,))
                    out(self.s_rx[b, 1], c, t0, o)

    def phase_C2(self, l):
        kb, cfg = self.kb, self.cfg
        op = kb.op
        T = cfg.T
        identf = self.cf[:, 128:256]
        with ExitStack() as st:
            OH = self.sb(st, "OHs", [128, 64, 128], F32)
            kb.load(OH[:], self.cst_sel[:, :, :], OH)
            S = self.sb(st, "Sst", [128, 8, 64], F32)
            op('pool', lambda E: E.memset(S[:], 0.0), w=(S,))
            fm = [self.sb(st, "fm", [128, 20, 128], F32) for _ in range(2)]
            tm = [self.sb(st, "tm", [128, 5, 512], F32) for _ in range(2)]
            vt = [self.sb(st, "vt", [128, 8, 64], F32) for _ in range(2)]
            yt = [self.sb(st, "yt", [128, 8, 64], F32) for _ in range(2)]
            T1 = self.sb(st, "T1", [128, 8, 64], F32)
            T2 = self.sb(st, "T2", [128, 8, 64], F32)
            sa = self.sb(st, "sa", [128, 8], F32)
            pX = self.ps[0:5]
            pT = self.ps[5]
            for jb in range(T // 64):
                t0 = jb * 64
                f_, tm_, v_, y_ = fm[jb % 2], tm[jb % 2], vt[jb % 2], yt[jb % 2]
                for bb in range(2):
                    kb.load(f_[:, :, 64 * bb:64 * bb + 64], self.s_rv[bb].rearrange("x (c p) t -> p (x c) t", p=128)[:, :, t0:t0 + 64], f_)
                    kb.load(v_[64 * bb:64 * bb + 64, :, :], self.s_rx[bb, 0].rearrange("(h p) t -> p h t", p=64)[:, :, t0:t0 + 64], v_)
                for x in range(5):
                    for c in range(4):
                        op('pe', lambda E: E.transpose(out=pT[:, c * 128:c * 128 + 128], in_=f_[:, x * 4 + c, :], identity=identf), r=(f_, self.cf), w=(pT,))
                    op('act', lambda E: E.activation(out=tm_[:, x, :], in_=pT[:], func=AF.Copy), r=(pT,), w=(tm_,))
                for j in range(64):
                    for x in range(5):
                        op('pe', lambda E: E.matmul(pX[x][:], lhsT=OH[:, j, :], rhs=tm_[:, x, :], start=True, stop=True), r=(OH, tm_), w=(pX[x],))
                    R_, W_, K_, KK, B_ = [p[:].rearrange("p (h k) -> p h k", h=8) for p in pX]
                    op('dve', lambda E: E.tensor_tensor(out=T1[:], in0=S[:], in1=KK, op=ALU.mult), r=(S, pX[3]), w=(T1,))
                    op('dve', lambda E: E.tensor_reduce(out=sa[:], in_=T1[:], axis=AX.X, op=ALU.add), r=(T1,), w=(sa,))
                    op('dve', lambda E: E.tensor_tensor(out=S[:], in0=S[:], in1=W_, op=ALU.mult), r=(S, pX[1]), w=(S,))
                    op('dve', lambda E: E.tensor_tensor(out=T2[:], in0=B_, in1=sa[:].unsqueeze(2).to_broadcast([128, 8, 64]), op=ALU.mult), r=(pX[4], sa), w=(T2,))
                    op('dve', lambda E: E.tensor_tensor(out=S[:], in0=S[:], in1=T2[:], op=ALU.subtract), r=(S, T2), w=(S,))
                    op('dve', lambda E: E.tensor_tensor(out=T1[:], in0=K_, in1=v_[:, :, j:j + 1].to_broadcast([128, 8, 64]), op=ALU.mult), r=(pX[2], v_), w=(T1,))
                    op('dve', lambda E: E.tensor_tensor(out=S[:], in0=S[:], in1=T1[:], op=ALU.add), r=(S, T1), w=(S,))
                    op('dve', lambda E: E.tensor_tensor(out=T2[:], in0=S[:], in1=R_, op=ALU.mult), r=(S, pX[0]), w=(T2,))
                    op('dve', lambda E: E.tensor_reduce(out=y_[:, :, j], in_=T2[:], axis=AX.X, op=ALU.add), r=(T2,), w=(y_,))
                for bb in range(2):
                    kb.store(self.s_ry[bb].rearrange("(h p) t -> p h t", p=64)[:, :, t0:t0 + 64], y_[64 * bb:64 * bb + 64, :, :], y_, q='sp')

    def phase_C3(self, l, b):
        kb, cfg = self.kb, self.cfg
        op = kb.op
        T = cfg.T
        P0 = PV_C
        LG, LB = P0 + 34, P0 + 38
        with ExitStack() as st:
            pv = self.sb(st, "pv", [128, NPV], F32)
            kb.load(pv[:], self.pvec[l], pv)
            bd = self.sb(st, "bd", [128, 128], F32)
            kb.load(bd[:], self.cst_bd[:, :], bd)
            epsg = self.sb(st, "epsg", [128, 1], F32)
            op('pool', lambda E: E.memset(epsg[:], 64e-5), w=(epsg,))
            yy = [self.sb(st, "yy", [128, 512], F32) for _ in range(2)]
            bb_ = [self.sb(st, "bon", [128, 512], F32) for _ in range(2)]
            gg = [self.sb(st, "ggc", [128, 512], F32) for _ in range(2)]
            yc = [self.sb(st, "yc", [128, 512], F32) for _ in range(2)]
            sq = [self.sb(st, "ysq3", [128, 512], F32) for _ in range(2)]
            ob = [self.sb(st, "ob3", [128, 512], BF16) for _ in range(2)]
            n = 0
            for tb in range(T // 512):
                t0 = tb * 512
                for c in range(4):
                    k = n % 2
                    n += 1
                    rows = slice(c * 128, c * 128 + 128)
                    kb.load(yy[k][:], self.s_ry[b][rows, t0:t0 + 512], yy[k])
                    kb.load(bb_[k][:], self.s_rx[b, 2][rows, t0:t0 + 512], bb_[k])
                    kb.load(gg[k][:], self.s_rx[b, 1][rows, t0:t0 + 512], gg[k])
                    pm = self.nps()
                    op('pe', lambda E: E.matmul(pm[:], lhsT=bd[:], rhs=yy[k][:], start=True, stop=True), r=(bd, yy[k]), w=(pm,))
                    op('dve', lambda E: E.scalar_tensor_tensor(out=yc[k][:], in0=pm[:], scalar=-1.0 / 64, in1=yy[k][:], op0=ALU.mult, op1=ALU.add), r=(pm, yy[k]), w=(yc[k],))
                    op('act', lambda E: E.activation(out=sq[k][:], in_=yc[k][:], func=AF.Square), r=(yc[k],), w=(sq[k],))
                    pv_ = self.nps()
                    op('pe', lambda E: E.matmul(pv_[:], lhsT=bd[:], rhs=sq[k][:], start=True, stop=True), r=(bd, sq[k]), w=(pv_,))
                    op('act', lambda E: E.activation(out=sq[k][:], in_=pv_[:], func=AF.Sqrt, bias=epsg[:], scale=1.0 / 64), r=(pv_, epsg), w=(sq[k],))
                    op('dve', lambda E: E.reciprocal(out=sq[k][:], in_=sq[k][:]), r=(sq[k],), w=(sq[k],))
                    op('dve', lambda E: E.tensor_tensor(out=yc[k][:], in0=yc[k][:], in1=sq[k][:], op=ALU.mult), r=(yc[k], sq[k]), w=(yc[k],))
                    op('dve', lambda E: E.tensor_scalar(out=yc[k][:], in0=yc[k][:], scalar1=pv[:, LG + c:LG + c + 1], scalar2=pv[:, LB + c:LB + c + 1], op0=ALU.mult, op1=ALU.add), r=(yc[k], pv), w=(yc[k],))
                    op('pool', lambda E: E.tensor_tensor(out=yc[k][:], in0=yc[k][:], in1=bb_[k][:], op=ALU.add), r=(yc[k], bb_[k]), w=(yc[k],))
                    op('pool', lambda E: E.tensor_tensor(out=ob[k][:], in0=yc[k][:], in1=gg[k][:], op=ALU.mult), r=(yc[k], gg[k]), w=(ob[k],))
                    kb.store(self.s_y4[b, 2][rows, t0:t0 + 512], ob[k][:], ob[k], q='sp')


def make_consts():
    c = np.zeros((128, 512), np.float32)
    c[:, 0:128] = 1.0
    c[:, 128:256] = np.eye(128, dtype=np.float32)
    c[:, 256:384] = np.triu(np.ones((128, 128), np.float32))
    c[:, 384] = EPS
    c[:, 385] = -np.pi
    return c


def tile_vec(v):
    return np.ascontiguousarray(v.reshape(-1, 128).T)


def bc(v):
    return np.broadcast_to(np.asarray(v, np.float32).reshape(1, -1), (128, np.asarray(v).size))


def make_pvec(inp, cfg):
    NL = cfg.NL
    pv = np.zeros((NL, 128, NPV), np.float32)
    for l in range(NL):
        p = pv[l]
        cw = inp["ssd_conv_w"][l]
        for i in range(9):
            r0, n = (128 * i, 128) if i < 4 else ((512 + 64 * (i - 4), 64) if i < 8 else (512, 128))
            for j in range(4):
                p[0:n, PV_A + i * 4 + j] = cw[j, r0:r0 + n]
            p[0:n, PV_A + 36 + i] = inp["ssd_conv_b"][l][r0:r0 + n]
        p[:, PV_A + 48:PV_A + 56] = bc(inp["ssd_dt_bias"][l])
        p[:, PV_A + 56:PV_A + 64] = bc(inp["ssd_a_log"][l])
        p[:, PV_A + 64:PV_A + 72] = bc(inp["ssd_d"][l])
        p[0:64, PV_A + 72:PV_A + 80] = inp["ssd_norm_g"][l].reshape(8, 64).T
        st_ = lambda a: np.ascontiguousarray(a.reshape(16, 128).T)
        p[:, PV_D:PV_D + 16] = st_(inp["s5_a_re"][l])
        p[:, PV_D + 16:PV_D + 32] = st_(inp["s5_a_im"][l])
        p[:, PV_D + 32:PV_D + 48] = st_(np.repeat(inp["s5_log_dt"][l][:, None], 64, 1))
        p[:, PV_D + 48:PV_D + 52] = tile_vec(inp["s5_d"][l])
        p[:, PV_D + 52:PV_D + 56] = tile_vec(inp["s5_glu_b"][l])
        p[:, PV_C:PV_C + 14] = tile_vec(inp["rwkv_mu"][l])
        p[:, PV_C + 14:PV_C + 18] = tile_vec(inp["rwkv_w0"][l])
        p[:, PV_C + 18:PV_C + 22] = tile_vec(inp["rwkv_a0"][l])
        p[:, PV_C + 22:PV_C + 26] = tile_vec(inp["rwkv_k_k"][l])
        p[:, PV_C + 26:PV_C + 30] = tile_vec(inp["rwkv_k_a"][l])
        p[:, PV_C + 30:PV_C + 34] = tile_vec(inp["rwkv_r_k"][l].reshape(-1))
        p[:, PV_C + 34:PV_C + 38] = tile_vec(inp["rwkv_ln_g"][l])
        p[:, PV_C + 38:PV_C + 42] = tile_vec(inp["rwkv_ln_b"][l])
    return pv


def make_s5w(inp, cfg):
    NL = cfg.NL
    wb = np.zeros((NL, 2, 16, 128, 128), np.float32)
    wc = np.zeros((NL, 2, 16, 128, 128), np.float32)
    for l in range(NL):
        for r, (bk, ck) in enumerate((("s5_b_re", "s5_c_re"), ("s5_b_im", "s5_c_im"))):
            bb, cc = inp[bk][l], inp[ck][l]
            for g in range(32):
                i, half, gl = g // 2, g % 2, g % 8
                wb[l, r, i, gl * 16:gl * 16 + 16, half * 64:half * 64 + 64] = bb[g].T
                wc[l, r, i, half * 64:half * 64 + 64, gl * 16:gl * 16 + 16] = cc[g].T
    return wb, wc


def make_rw_consts():
    bd = np.zeros((128, 128), np.float32)
    bd[0:64, 0:64] = 1.0
    bd[64:128, 64:128] = 1.0
    sel = np.zeros((128, 64, 128), np.float32)
    for bb in range(2):
        for j in range(64):
            sel[64 * bb + j, j, 64 * bb:64 * bb + 64] = 1.0
    return bd, sel


def host_inputs(inp, core, cfg):
    NL, T = cfg.NL, cfg.T
    m = {}
    xs = inp["x"][2 * core:2 * core + 2, :T]
    m["xT"] = np.ascontiguousarray(xs.transpose(0, 2, 1))
    for k in ("w_in", "w_branch", "w_out", "ffn_w1", "ffn_w3", "ffn_w2"):
        m[k] = np.ascontiguousarray(inp[k][:NL])
    gv = np.zeros((128, NL * 16 + 8), np.float32)
    for l in range(NL):
        gv[:, l * 16:l * 16 + 8] = tile_vec(inp["norm_mix_g"][l])
        gv[:, l * 16 + 8:l * 16 + 16] = tile_vec(inp["norm_ffn_g"][l])
    gv[:, NL * 16:] = tile_vec(inp["norm_final_g"])
    m["gvec"] = gv
    m["cst_f32"] = make_consts()
    m["pvec"] = make_pvec(inp, cfg)
    m["cst_l"] = np.ascontiguousarray(np.broadcast_to(np.arange(512, dtype=np.float32), (128, 512)))
    m["s5wb"], m["s5wc"] = make_s5w(inp, cfg)
    m["s5glu"] = np.ascontiguousarray(inp["s5_glu_w"][:cfg.NL])
    m["cst_oh"] = OH_TILES
    m["cst_bd"], m["cst_sel"] = make_rw_consts()
    for k_, n_ in (("rw_w2", "rwkv_w2"), ("rw_a2", "rwkv_a2"), ("rw_g2", "rwkv_g2")):
        m[k_] = np.ascontiguousarray(inp[n_][:cfg.NL])
    m["relb"] = np.ascontiguousarray(bc(inp["rel_bias"].reshape(-1)))
    return m


def kernel(**inp):
    cfg = Cfg()
    inp = {k: np.asarray(v) for k, v in inp.items()}
    nc, B = build(cfg)
    in_maps = [host_inputs(inp, c, cfg) for c in range(8)]
    res = run_bass_kernel_spmd(nc, in_maps, core_ids=list(range(8)))
    out = np.zeros((16, cfg.T, D), np.float32)
    for c in range(8):
        out[2 * c:2 * c + 2] = res.results[c]["outT"].transpose(0, 2, 1)
    return out
```
